# Optimizing a Trainium2 kernel written in Bass

```python
import math
import jax, jax.numpy as jnp
from jax import lax
import numpy as np

D_MODEL = 1024
BATCH = 4
SEQ = 4096
DEPTH = 4

GRID_W = 64
CTX_LEN = 256
N_EVEN = (DEPTH + 1) // 2
N_ODD = DEPTH // 2

NA_HEADS = 8
HEAD_DIM = D_MODEL // (2 * NA_HEADS)
NA_WIDTH = NA_HEADS * HEAD_DIM
NA_WIN_H = 8
NA_WIN_W = 16
SC_WIDTH = D_MODEL - NA_WIDTH
SC_CONV = 3
HY_WIDTH = D_MODEL // 2
HY_ORDER = 2
HY_SHORT = 3
HY_BANDS = 16
HY_EMB = 1 + 2 * HY_BANDS
HY_FFN = 64
HY_TARGET = 1e-2
HY_FAST = 0.3
HY_SLOW = 1.5
HY_MAX_DECAY = math.log(HY_TARGET) / HY_FAST
HY_MIN_DECAY = math.log(HY_TARGET) / HY_SLOW
CF_WIDTH = D_MODEL - HY_WIDTH
CF_CONV_WIDTH = 31
EVEN_IN = 3 * NA_WIDTH + 3 * SC_WIDTH
ODD_IN = 3 * HY_WIDTH + 2 * CF_WIDTH
N_GROUPS = 4
EXPERTS_PER_GROUP = 4
N_EXPERTS = N_GROUPS * EXPERTS_PER_GROUP
TOP_K = 2
D_EXPERT = D_MODEL // 4

RMS_EPS = 1e-6
LN_EPS = 1e-5
NEG_INF = -1e30

kernel_name = 'hybrid_natten_hyena_conformer_hmoe_dit'


def rmsnorm(x, g):
    xf = x.astype(jnp.float32)
    y = xf * lax.rsqrt(jnp.mean(xf * xf, axis=-1, keepdims=True) + RMS_EPS)
    return (y * g).astype(x.dtype)


def layernorm(x, g, b):
    xf = x.astype(jnp.float32)
    mu = jnp.mean(xf, axis=-1, keepdims=True)
    var = jnp.mean(jnp.square(xf - mu), axis=-1, keepdims=True)
    return ((xf - mu) * lax.rsqrt(var + LN_EPS) * g + b).astype(x.dtype)


def split_cols(p, sizes):
    return jnp.split(p, [int(i) for i in np.cumsum(sizes)[:-1]], axis=-1)


def dwconv(x, w):
    k = w.shape[0]
    return lax.conv_general_dilated(
        x, w[:, None, :], window_strides=(1,), padding=[(k // 2, k - 1 - k // 2)],
        dimension_numbers=('NWC', 'WIO', 'NWC'), feature_group_count=x.shape[-1])


def heads(a):
    return a.reshape(a.shape[0], a.shape[1], NA_HEADS, HEAD_DIM)


def neighbourhood_attention(q, k, v, k_ctx, v_ctx, rpb):
    b, n, h, dh = q.shape
    rows = n // GRID_W
    kh = min(NA_WIN_H, rows)
    kw = NA_WIN_W
    scale = dh ** -0.5
    qg = q.reshape(b, rows, GRID_W, h, dh)
    kg = k.reshape(b, rows, GRID_W, h, dh)
    vg = v.reshape(b, rows, GRID_W, h, dh)
    cols = jnp.arange(GRID_W)
    col_start = jnp.clip(cols - kw // 2, 0, GRID_W - kw)
    col_valid = (cols[None, :] >= col_start[:, None]) & (cols[None, :] < col_start[:, None] + kw)
    col_idx = jnp.clip(cols[None, :] - cols[:, None], -(kw - 1), kw - 1) + (NA_WIN_W - 1)

    def row_block(args):
        q_row, r = args
        start = jnp.clip(r - kh // 2, 0, rows - kh)
        k_band = lax.dynamic_slice_in_dim(kg, start, kh, axis=1)
        v_band = lax.dynamic_slice_in_dim(vg, start, kh, axis=1)
        row_idx = start + jnp.arange(kh) - r + (NA_WIN_H - 1)
        bias = rpb[:, row_idx[None, :, None], col_idx[:, None, :]]
        s_win = jnp.einsum('bqhd,brkhd->bhqrk', q_row, k_band).astype(jnp.float32) * scale + bias
        s_win = jnp.where(col_valid[:, None, :], s_win, NEG_INF)
        s_ctx = jnp.einsum('bqhd,bchd->bhqc', q_row, k_ctx).astype(jnp.float32) * scale
        s = jnp.concatenate([s_win.reshape(b, h, GRID_W, kh * GRID_W), s_ctx], axis=-1)
        p = jax.nn.softmax(s, axis=-1).astype(v.dtype)
        p_win = p[..., :kh * GRID_W].reshape(b, h, GRID_W, kh, GRID_W)
        p_ctx = p[..., kh * GRID_W:]
        return (jnp.einsum('bhqrk,brkhd->bqhd', p_win, v_band)
                + jnp.einsum('bhqc,bchd->bqhd', p_ctx, v_ctx))

    out = lax.map(row_block, (jnp.moveaxis(qg, 1, 0), jnp.arange(rows)))
    return jnp.moveaxis(out, 0, 1).reshape(b, n, h * dh)


def context_attention(q, k, v):
    b, lc, h, dh = q.shape
    s = jnp.einsum('bqhd,bkhd->bhqk', q, k).astype(jnp.float32) * dh ** -0.5
    p = jax.nn.softmax(s, axis=-1).astype(v.dtype)
    return jnp.einsum('bhqk,bkhd->bqhd', p, v).reshape(b, lc, h * dh)


def short_gated_conv(gb, gc, hv, w):
    return gb * dwconv(gc * hv, w)


def even_mixer(u_lat, u_ctx, w_in, qn_g, kn_g, rpb, sc_w, ctx_needed):
    sizes = [NA_WIDTH] * 3 + [SC_WIDTH] * 3

    def project(u):
        q, k, v, gb, gc, hv = split_cols(u @ w_in, sizes)
        return rmsnorm(heads(q), qn_g), rmsnorm(heads(k), kn_g), heads(v), gb, gc, hv

    ql, kl, vl, bl, cl, hl = project(u_lat)
    if ctx_needed:
        qc, kc, vc, bc, cc, hc = project(u_ctx)
    else:
        kc, vc = split_cols(u_ctx @ w_in[:, NA_WIDTH:3 * NA_WIDTH], [NA_WIDTH, NA_WIDTH])
        kc, vc = rmsnorm(heads(kc), kn_g), heads(vc)
    y_lat = jnp.concatenate([neighbourhood_attention(ql, kl, vl, kc, vc, rpb),
                             short_gated_conv(bl, cl, hl, sc_w)], axis=-1)
    if not ctx_needed:
        return y_lat, None
    y_ctx = jnp.concatenate([context_attention(qc, kc, vc),
                             short_gated_conv(bc, cc, hc, sc_w)], axis=-1)
    return y_lat, y_ctx


def hyena_filter_bank(length, w1, b1, w2, b2, w3, freq):
    f32 = jnp.float32
    t = jnp.linspace(0.0, 1.0, length, dtype=f32)[:, None]
    w = 2.0 * math.pi * jnp.arange(length, dtype=f32)[:, None] / length
    bands = jnp.linspace(1e-4, HY_BANDS - 1, HY_BANDS, dtype=f32)[None, :]
    z = jnp.concatenate([t, jnp.cos(bands * w), -jnp.sin(bands * w)], axis=-1)
    hid = jnp.sin(freq[0] * (z @ w1.astype(f32) + b1))
    hid = jnp.sin(freq[1] * (hid @ w2.astype(f32) + b2))
    filt = (hid @ w3.astype(f32)).reshape(length, HY_ORDER, 2, HY_WIDTH)
    deltas = jnp.abs(jnp.linspace(HY_MIN_DECAY, HY_MAX_DECAY, HY_WIDTH, dtype=f32))
    filt = filt * jnp.exp(-t * deltas)[:, None, None, :]
    filt = filt / jnp.sum(jnp.abs(filt), axis=(0, 2), keepdims=True)
    fwd, bwd = filt[:, :, 0], filt[:, :, 1]
    taps = jnp.concatenate([fwd, jnp.zeros_like(fwd[:1]), bwd[:0:-1]], axis=0)
    return jnp.fft.rfft(taps, axis=0)


def long_conv(u, taps_f, bias):
    n = u.shape[1]
    uf = u.astype(jnp.float32)
    y = jnp.fft.irfft(jnp.fft.rfft(uf, n=2 * n, axis=1) * taps_f, n=2 * n, axis=1)[:, :n]
    return (y + uf * bias).astype(u.dtype)


def odd_mixer(u, w_in, short_w, w1, b1, w2, b2, w3, freq, hy_bias, cf_w, cf_b, ln_g, ln_b):
    hy, a, g = split_cols(u @ w_in, [3 * HY_WIDTH, CF_WIDTH, CF_WIDTH])
    v, x1, x2 = split_cols(dwconv(hy, short_w), [HY_WIDTH] * 3)
    taps_f = hyena_filter_bank(u.shape[1], w1, b1, w2, b2, w3, freq)
    z = v
    for o, gate in enumerate((x1, x2)):
        z = gate * long_conv(z, taps_f[:, o], hy_bias[o])
    cf = dwconv(a * jax.nn.sigmoid(g), cf_w) + cf_b
    cf = jax.nn.silu(layernorm(cf, ln_g, ln_b))
    return jnp.concatenate([z, cf], axis=-1)


def hier_moe(h, w_group, b_group, w_router, b_router, w_gate, w_up, w_down):
    b, n, d = h.shape
    t = h.reshape(b * n, d)
    tf = t.astype(jnp.float32)
    g_logits = tf @ w_group.astype(jnp.float32) + b_group
    g_prob = jax.nn.softmax(g_logits, axis=-1)
    g_idx = jnp.argmax(g_logits, axis=-1)
    g_w = jnp.take_along_axis(g_prob, g_idx[:, None], axis=-1)
    e_logits = (tf @ w_router.astype(jnp.float32) + b_router).reshape(-1, N_GROUPS, EXPERTS_PER_GROUP)
    e_logits = jnp.take_along_axis(e_logits, g_idx[:, None, None], axis=1)[:, 0]
    top_v, top_i = lax.top_k(e_logits, TOP_K)
    top_w = jax.nn.softmax(top_v, axis=-1) * g_w
    expert_id = g_idx[:, None] * EXPERTS_PER_GROUP + top_i
    combine = jnp.sum(jax.nn.one_hot(expert_id, N_EXPERTS, dtype=jnp.float32) * top_w[..., None],
                      axis=1).astype(t.dtype)
    act = (jax.nn.silu(jnp.einsum('td,edf->tef', t, w_gate))
           * jnp.einsum('td,edf->tef', t, w_up) * combine[..., None])
    return jnp.einsum('tef,efd->td', act, w_down).reshape(b, n, d)


def setup_inputs(seed: int = 0) -> dict:
    key = jax.random.key(seed)
    ks = iter(jax.random.split(key, 48))
    f32 = jnp.float32
    D = D_MODEL

    def nrm(shape, scale):
        return jax.random.normal(next(ks), shape, f32) * scale

    def gain(shape):
        return 1.0 + nrm(shape, 0.02)

    return {
        'x': nrm((BATCH, SEQ, D), 1.0),
        'c': nrm((BATCH, D), 1.0),
        'ctx': nrm((BATCH, CTX_LEN, D), 1.0),
        'c_ctx': nrm((D,), 1.0),
        'ada_w': nrm((DEPTH, D, 6 * D), 0.5 * D ** -0.5),
        'ada_b': nrm((DEPTH, 6 * D), 0.02),
        'norm1_g': gain((DEPTH, D)),
        'norm2_g': gain((DEPTH, D)),
        'w_in_even': nrm((N_EVEN, D, EVEN_IN), D ** -0.5),
        'qn_g': gain((N_EVEN, HEAD_DIM)),
        'kn_g': gain((N_EVEN, HEAD_DIM)),
        'na_rpb': nrm((N_EVEN, NA_HEADS, 2 * NA_WIN_H - 1, 2 * NA_WIN_W - 1), 0.1),
        'sc_conv_w': nrm((N_EVEN, SC_CONV, SC_WIDTH), SC_CONV ** -0.5),
        'w_in_odd': nrm((N_ODD, D, ODD_IN), D ** -0.5),
        'hy_short_w': nrm((N_ODD, HY_SHORT, 3 * HY_WIDTH), HY_SHORT ** -0.5),
        'hy_w1': nrm((N_ODD, HY_EMB, HY_FFN), HY_EMB ** -0.5),
        'hy_b1': nrm((N_ODD, HY_FFN), 0.1),
        'hy_w2': nrm((N_ODD, HY_FFN, HY_FFN), HY_FFN ** -0.5),
        'hy_b2': nrm((N_ODD, HY_FFN), 0.1),
        'hy_w3': nrm((N_ODD, HY_FFN, HY_ORDER * 2 * HY_WIDTH), HY_FFN ** -0.5),
        'hy_freq': 1.0 + nrm((N_ODD, 2, HY_FFN), 0.1),
        'hy_bias': nrm((N_ODD, HY_ORDER, HY_WIDTH), 0.5),
        'cf_conv_w': nrm((N_ODD, CF_CONV_WIDTH, CF_WIDTH), CF_CONV_WIDTH ** -0.5),
        'cf_conv_b': nrm((N_ODD, CF_WIDTH), 0.02),
        'cf_ln_g': gain((N_ODD, CF_WIDTH)),
        'cf_ln_b': nrm((N_ODD, CF_WIDTH), 0.02),
        'w_out': nrm((DEPTH, D, D), D ** -0.5),
        'moe_w_group': nrm((DEPTH, D, N_GROUPS), D ** -0.5),
        'moe_b_group': nrm((DEPTH, N_GROUPS), 0.01),
        'moe_w_router': nrm((DEPTH, D, N_EXPERTS), D ** -0.5),
        'moe_b_router': nrm((DEPTH, N_EXPERTS), 0.01),
        'moe_w_gate': nrm((DEPTH, N_EXPERTS, D, D_EXPERT), D ** -0.5),
        'moe_w_up': nrm((DEPTH, N_EXPERTS, D, D_EXPERT), D ** -0.5),
        'moe_w_down': nrm((DEPTH, N_EXPERTS, D_EXPERT, D), D_EXPERT ** -0.5),
    }


def reference(x, c, ctx, c_ctx, ada_w, ada_b, norm1_g, norm2_g,
              w_in_even, qn_g, kn_g, na_rpb, sc_conv_w,
              w_in_odd, hy_short_w, hy_w1, hy_b1, hy_w2, hy_b2, hy_w3, hy_freq, hy_bias,
              cf_conv_w, cf_conv_b, cf_ln_g, cf_ln_b, w_out,
              moe_w_group, moe_b_group, moe_w_router, moe_b_router,
              moe_w_gate, moe_w_up, moe_w_down):
    for l in range(DEPTH):
        ctx_needed = any(j % 2 == 0 for j in range(l + 1, DEPTH))
        sh1, sc1, g1, sh2, sc2, g2 = [m[:, None, :] for m in
                                      jnp.split(jax.nn.silu(c) @ ada_w[l] + ada_b[l], 6, axis=-1)]
        csh1, csc1, cg1, csh2, csc2, cg2 = jnp.split(jax.nn.silu(c_ctx) @ ada_w[l] + ada_b[l], 6, axis=-1)
        moe_p = (moe_w_group[l], moe_b_group[l], moe_w_router[l], moe_b_router[l],
                 moe_w_gate[l], moe_w_up[l], moe_w_down[l])
        u_lat = rmsnorm(x, norm1_g[l]) * (1.0 + sc1) + sh1
        if l % 2 == 0 or ctx_needed:
            u_ctx = rmsnorm(ctx, norm1_g[l]) * (1.0 + csc1) + csh1
        if l % 2 == 0:
            e = l // 2
            y_lat, y_ctx = even_mixer(u_lat, u_ctx, w_in_even[e], qn_g[e], kn_g[e], na_rpb[e],
                                      sc_conv_w[e], ctx_needed)
        else:
            o = l // 2
            odd_p = (w_in_odd[o], hy_short_w[o], hy_w1[o], hy_b1[o], hy_w2[o], hy_b2[o], hy_w3[o],
                     hy_freq[o], hy_bias[o], cf_conv_w[o], cf_conv_b[o], cf_ln_g[o], cf_ln_b[o])
            y_lat = odd_mixer(u_lat, *odd_p)
            y_ctx = odd_mixer(u_ctx, *odd_p) if ctx_needed else None
        x = x + g1 * (y_lat @ w_out[l])
        x = x + g2 * hier_moe(rmsnorm(x, norm2_g[l]) * (1.0 + sc2) + sh2, *moe_p)
        if ctx_needed:
            ctx = ctx + cg1 * (y_ctx @ w_out[l])
            ctx = ctx + cg2 * hier_moe(rmsnorm(ctx, norm2_g[l]) * (1.0 + csc2) + csh2, *moe_p)
    return x
```

```python
import math
import numpy as np
import concourse.bass as bass
import concourse.mybir as mybir
from concourse.bass_utils import run_bass_kernel_spmd

F32 = mybir.dt.float32
BF16 = mybir.dt.bfloat16
AF = mybir.ActivationFunctionType
ALU = mybir.AluOpType
AX = mybir.AxisListType

D_MODEL = 1024
BATCH = 4
SEQ = 4096
DEPTH = 4
GRID_W = 64
CTX_LEN = 256
NCORES = 8
RMS_EPS = 1e-6
LN_EPS = 1e-5
BIG = 1.0e30

ENGS = ("sync", "scalar", "vector", "gpsimd", "tensor")


class Tl:
    __slots__ = ("h", "name", "last_w", "readers")

    def __init__(self, h, name):
        self.h = h
        self.name = name
        self.last_w = None
        self.readers = []

    def __getitem__(self, idx):
        return self.h[idx]


class Prog:
    def __init__(self):
        self.nc = bass.Bass("TRN2", target_bir_lowering=False)
        self.ops = {e: [] for e in ENGS}
        self.ndma = 0
        self.out_dmas = []

    def sb(self, name, shape, dt=F32):
        return Tl(self.nc.alloc_sbuf_tensor(name, list(shape), dt), name)

    def ps(self, name, shape=(128, 512), dt=F32):
        return Tl(self.nc.alloc_psum_tensor(name, list(shape), dt), name)

    def din(self, name, shape, dt=F32):
        return Tl(self.nc.dram_tensor(name, list(shape), dt, kind="ExternalInput").ap(), name)

    def dout(self, name, shape, dt=F32):
        return Tl(self.nc.dram_tensor(name, list(shape), dt, kind="ExternalOutput").ap(), name)

    def dtmp(self, name, shape, dt=F32):
        return Tl(self.nc.dram_tensor(name, list(shape), dt, kind="Internal").ap(), name)

    def op(self, eng, fn, reads=(), writes=(), dma=False):
        deps = set()
        me = (eng, len(self.ops[eng]))
        for t in reads:
            if t.last_w is not None:
                deps.add(t.last_w)
        for t in writes:
            if t.last_w is not None:
                deps.add(t.last_w)
            for r in t.readers:
                deps.add(r)
        deps.discard(me)
        for t in reads:
            t.readers.append(me)
        for t in writes:
            t.last_w = me
            t.readers = []
        rec = dict(fn=fn, deps=deps, dma=dma, dma_id=None)
        if dma:
            rec["dma_id"] = self.ndma
            self.ndma += 1
        self.ops[eng].append(rec)
        return me

    def fence(self, from_tls, to_tls):
        pend = []
        for t in from_tls:
            if t.last_w is not None:
                pend.append(t.last_w)
            pend.extend(t.readers)
        for t in to_tls:
            t.readers.extend(pend)

    def view(self, name, ap):
        return Tl(ap, name)

    def dma(self, out, in_, reads, writes, eng="sync", **kw):
        return self.op(eng, lambda e: e.dma_start(out=out, in_=in_, **kw), reads, writes, dma=True)

    def mm(self, out, lhsT, rhs, reads, writes, start=True, stop=True):
        return self.op("tensor", lambda e: e.matmul(out, lhsT, rhs, start=start, stop=stop), reads, writes)

    def v(self, fn, reads, writes):
        return self.op("vector", fn, reads, writes)

    def s(self, fn, reads, writes):
        return self.op("scalar", fn, reads, writes)

    def g(self, fn, reads, writes):
        return self.op("gpsimd", fn, reads, writes)

    def emit(self):
        nc = self.nc
        NDS = 24
        sems = {e: nc.alloc_semaphore(name=f"s_{e}") for e in ENGS}
        dsems = [nc.alloc_semaphore(name=f"d_{i}") for i in range(NDS)]
        ops = self.ops
        dma_by_id = {}
        for e in ENGS:
            c = 0
            for r in ops[e]:
                if r["dma"]:
                    k = r["dma_id"]
                    r["ev"] = (dsems[k % NDS], 16 * (k // NDS + 1))
                    dma_by_id[k] = r
                else:
                    c += 1
                    r["ev"] = (sems[e], c)

        def run_engine(e, eng):
            waited = {}

            def wait(ev):
                s, v = ev
                if waited.get(s.name, 0) >= v:
                    return
                eng.wait_ge(s, v)
                waited[s.name] = v

            for r in ops[e]:
                for (de, di) in sorted(r["deps"]):
                    dr = ops[de][di]
                    if de == e and e == "tensor" and not dr["dma"]:
                        continue
                    wait(dr["ev"])
                if r["dma"]:
                    k = r["dma_id"]
                    if k >= NDS:
                        wait(dma_by_id[k - NDS]["ev"])
                    r["fn"](eng).then_inc(r["ev"][0], 16)
                else:
                    r["fn"](eng).then_inc(r["ev"][0], 1)
            for r in ops[e]:
                if r["dma"]:
                    wait(r["ev"])

        with nc.Block() as block:
            @block.sync
            def _(eng):
                run_engine("sync", eng)

            @block.scalar
            def _(eng):
                run_engine("scalar", eng)

            @block.vector
            def _(eng):
                run_engine("vector", eng)

            @block.gpsimd
            def _(eng):
                run_engine("gpsimd", eng)

            @block.tensor
            def _(eng):
                run_engine("tensor", eng)
        return nc


def fm(ap, p=128):
    return ap.rearrange("(c p) t -> p c t", p=p)


def const_inputs():
    ident = np.eye(128, dtype=np.float32)
    sel = np.zeros((16, 16, 128), np.float32)
    for e in range(16):
        sel[e, e, :] = 1.0
    return {"c_ident": ident, "c_sel": sel}


class Consts:
    def __init__(self, P, need_sel=True):
        self.P = P
        d_ident = P.din("c_ident", [128, 128])
        self.ident = P.sb("ident", [128, 128])
        self.ones_bf = P.sb("ones_bf", [128, 128], BF16)
        P.dma(self.ident[:], d_ident[:], [d_ident], [self.ident])
        if need_sel:
            d_sel = P.din("c_sel", [16, 16, 128])
            self.sel = P.sb("sel", [16, 16, 128])
            P.dma(self.sel[:], d_sel[:].rearrange("e k m -> k e m"), [d_sel], [self.sel])
        P.v(lambda e: e.memset(self.ones_bf[:], 1.0), [], [self.ones_bf])
        self.eps_rms = P.sb("eps_rms", [128, 1])
        P.v(lambda e: e.memset(self.eps_rms[:], 1024.0 * RMS_EPS), [], [self.eps_rms])


def build_mod():
    P = Prog()
    NCH = 24
    cvT = P.din("cvT", [1024, 5])
    w = P.din("w", [1024, NCH * 128])
    b = P.din("b", [128, NCH])
    out = P.dout("out", [128, NCH, 5])
    cv = P.sb("cv", [128, 8, 5])
    bs = P.sb("bs", [128, NCH])
    res = P.sb("res", [128, NCH, 5])
    P.dma(cv[:], fm(cvT[:]), [cvT], [cv])
    P.dma(bs[:], b[:], [b], [bs])
    P.s(lambda e: e.activation(out=cv[:], in_=cv[:], func=AF.Silu), [cv], [cv])
    wt = [P.sb(f"wt{i}", [128, 8, 512]) for i in range(2)]
    pp = [P.ps(f"pp{i}", [128, 512]) for i in range(2)]
    for g in range(NCH // 4):
        t = wt[g % 2]
        P.dma(t[:], fm(w[:])[:, :, g * 512:(g + 1) * 512], [w], [t])
        for j in range(4):
            ch = g * 4 + j
            p = pp[ch % 2]
            for k in range(8):
                P.mm(p[:, 0:5], t[:, k, j * 128:(j + 1) * 128], cv[:, k, :], [t, cv], [p],
                     start=(k == 0), stop=(k == 7))
            P.s(lambda e, p=p, ch=ch: e.activation(out=res[:, ch, :], in_=p[:, 0:5], func=AF.Identity,
                                                   bias=bs[:, ch:ch + 1], scale=1.0), [p, bs], [res])
    P.dma(out[:], res[:], [res], [out])
    return P.emit()


class ModVecs:
    def __init__(self, P, n1g, n2g):
        self.P = P
        dL = P.din("modL", [128, 48])
        dC = P.din("modC", [128, 48])
        dn1 = P.din("n1g", [128, 8])
        dn2 = P.din("n2g", [128, 8])
        self.m = {}
        n1 = P.sb("n1g_s", [128, 8])
        n2 = P.sb("n2g_s", [128, 8])
        P.dma(n1[:], dn1[:], [dn1], [n1])
        P.dma(n2[:], dn2[:], [dn2], [n2])
        for nm, d in (("L", dL), ("C", dC)):
            t = P.sb("mods_" + nm, [128, 48])
            P.dma(t[:], d[:], [d], [t])
            gm1 = P.sb("gm1" + nm, [128, 8])
            gm2 = P.sb("gm2" + nm, [128, 8])
            P.v(lambda e, t=t, gm1=gm1: e.tensor_scalar(out=gm1[:], in0=t[:, 8:16], scalar1=1.0, scalar2=32.0,
                                                        op0=ALU.add, op1=ALU.mult), [t], [gm1])
            P.v(lambda e, gm1=gm1: e.tensor_tensor(out=gm1[:], in0=gm1[:], in1=n1[:], op=ALU.mult), [gm1, n1], [gm1])
            P.v(lambda e, t=t, gm2=gm2: e.tensor_scalar(out=gm2[:], in0=t[:, 32:40], scalar1=1.0, scalar2=32.0,
                                                        op0=ALU.add, op1=ALU.mult), [t], [gm2])
            P.v(lambda e, gm2=gm2: e.tensor_tensor(out=gm2[:], in0=gm2[:], in1=n2[:], op=ALU.mult), [gm2, n2], [gm2])
            self.m[nm] = dict(t=t, gm1=gm1, gm2=gm2)

    def get(self, which, name):
        d = self.m[which]
        t = d["t"]
        if name == "gm1":
            return d["gm1"], d["gm1"]
        if name == "gm2":
            return d["gm2"], d["gm2"]
        off = {"sh1": 0, "g1": 16, "sh2": 24, "g2": 40}[name]
        return t, t[:, off:off + 8]


def emit_norm_mod(P, C, x, xoff, n, gm, sh, ps_n, sq, rstd, tmp, out_f32=None, out_bf=None, ooff=0):
    gm_t, gm_ap = gm
    sh_t, sh_ap = sh
    for dc in range(8):
        P.s(lambda e, dc=dc: e.activation(out=sq[:, dc, 0:n], in_=x[:, dc, xoff:xoff + n], func=AF.Square),
            [x], [sq])
    for dc in range(8):
        P.mm(ps_n[:, 0:n], C.ones_bf[:], sq[:, dc, 0:n], [C.ones_bf, sq], [ps_n], start=(dc == 0), stop=(dc == 7))
    P.s(lambda e: e.activation(out=rstd[:, 0:n], in_=ps_n[:, 0:n], func=AF.Sqrt, bias=C.eps_rms[:, 0:1], scale=1.0),
        [ps_n, C.eps_rms], [rstd])
    P.v(lambda e: e.reciprocal(out=rstd[:, 0:n], in_=rstd[:, 0:n]), [rstd], [rstd])
    for dc in range(8):
        tm = tmp[dc % 2]
        P.v(lambda e, dc=dc, tm=tm: e.tensor_tensor(out=tm[:, 0:n], in0=x[:, dc, xoff:xoff + n], in1=rstd[:, 0:n],
                                                    op=ALU.mult), [x, rstd], [tm])
        if out_f32 is not None:
            P.s(lambda e, dc=dc, tm=tm: e.activation(out=out_f32[:, dc, ooff:ooff + n], in_=tm[:, 0:n],
                                                     func=AF.Identity, scale=gm_ap[:, dc:dc + 1],
                                                     bias=sh_ap[:, dc:dc + 1]),
                [tm, gm_t, sh_t], [out_f32])
            if out_bf is not None:
                P.v(lambda e, dc=dc: e.tensor_copy(out=out_bf[:, dc, ooff:ooff + n], in_=out_f32[:, dc, ooff:ooff + n]),
                    [out_f32], [out_bf])
        else:
            P.s(lambda e, dc=dc, tm=tm: e.activation(out=out_bf[:, dc, ooff:ooff + n], in_=tm[:, 0:n],
                                                     func=AF.Identity, scale=gm_ap[:, dc:dc + 1],
                                                     bias=sh_ap[:, dc:dc + 1]),
                [tm, gm_t, sh_t], [out_bf])


class PostRes:
    def __init__(self, P, C, PT):
        self.PT = PT
        self.d_wout = P.din("w_out", [1024, 1024])
        self.d_wr = P.din("w_r", [1024, 20])
        self.d_rb = P.din("r_b", [20])
        self.d_wg = P.din("w_gate", [16, 1024, 256])
        self.d_wu = P.din("w_up", [16, 1024, 256])
        self.d_wd = P.din("w_down", [16, 256, 1024])
        self.wout = P.sb("wout_s", [128, 8, 1024], BF16)
        self.wr = P.sb("wr_s", [128, 8, 20])
        self.rb = P.sb("rb_s", [128, 20])
        P.dma(self.wout[:], fm(self.d_wout[:]), [self.d_wout], [self.wout], eng="gpsimd")
        P.dma(self.wr[:], fm(self.d_wr[:]), [self.d_wr], [self.wr])
        P.dma(self.rb[:], self.d_rb[:].partition_broadcast(128), [self.d_rb], [self.rb])
        self.xp = P.sb("xp", [128, 8, PT])
        self.hb = P.sb("hb", [128, 8, PT], BF16)
        S = P.nc.alloc_sbuf_tensor("scratch", [128, 16 * PT], F32)
        self.act = P.view("act", S[:, :].bitcast(BF16).rearrange("p (c n) -> p c n", c=32))
        self.hf = P.view("hf", S[:, 0:4096].rearrange("p (c n) -> p c n", c=8))
        self.ybf = [P.view(f"ybf{i}", S[:, 4096 + 2048 * i:6144 + 2048 * i].bitcast(BF16)
                           .rearrange("p (c n) -> p c n", c=8)) for i in range(2)]
        self.sq = P.view("sq", S[:, 8192:10240].bitcast(BF16).rearrange("p (c n) -> p c n", c=8))
        self.tmp = [P.view(f"tmpn{i}", S[:, 10240 + 512 * i:10752 + 512 * i]) for i in range(2)]
        self.p0 = [self.hf, self.sq] + self.ybf + self.tmp
        self.rstd = P.sb("rstd", [128, 512])
        self.combT = P.sb("combT", [16, PT])
        self.wgu = [P.sb(f"wgu{i}", [128, 2, 8, 256], BF16) for i in range(2)]
        self.wd = [P.sb(f"wd{i}", [128, 32, 128], BF16) for i in range(2)]
        self.t1 = [P.sb(f"t1_{i}", [128, 512]) for i in range(2)]
        self.t2 = [P.sb(f"t2_{i}", [128, 512]) for i in range(2)]
        self.pw = [P.ps(f"pw{i}") for i in range(2)]
        self.pn = P.ps("pn")
        self.pcb = P.ps("pcb")
        self.pg = [P.ps(f"pg{i}") for i in range(2)]
        self.pu = [P.ps(f"pu{i}") for i in range(2)]
        self.r = {k: P.sb("r_" + k, [128, w]) for k, w in
                  dict(L=20, gmax=1, ngmax=1, gsum=1, gw=1, gmask=4, gexp=4, pen=4, elm=16, m1=1, mask1=16, elm2=16,
                       m2=1, mask2=16, d=1, w1=1, w2=1, comb=16).items()}


def emit_router_block(P, C, R, hf, hoff, coff):
    r = R.r
    pl = R.pcb
    for k in range(8):
        P.mm(pl[:, 0:20], hf[:, k, hoff:hoff + 128], R.wr[:, k, :], [hf, R.wr], [pl], start=(k == 0), stop=(k == 7))
    L = r["L"]
    P.v(lambda e: e.tensor_tensor(out=L[:], in0=pl[:, 0:20], in1=R.rb[:], op=ALU.add), [pl, R.rb], [L])
    P.v(lambda e: e.tensor_reduce(out=r["gmax"][:], in_=L[:, 0:4], axis=AX.X, op=ALU.max), [L], [r["gmax"]])
    P.v(lambda e: e.tensor_scalar(out=r["gmask"][:], in0=L[:, 0:4], scalar1=r["gmax"][:, 0:1], scalar2=None,
                                  op0=ALU.is_ge), [L, r["gmax"]], [r["gmask"]])
    P.v(lambda e: e.tensor_scalar(out=r["ngmax"][:], in0=r["gmax"][:], scalar1=-1.0, scalar2=None, op0=ALU.mult),
        [r["gmax"]], [r["ngmax"]])
    P.s(lambda e: e.activation(out=r["gexp"][:], in_=L[:, 0:4], func=AF.Exp, bias=r["ngmax"][:, 0:1], scale=1.0,
                               accum_out=r["gsum"][:, 0:1]), [L, r["ngmax"]], [r["gexp"], r["gsum"]])
    P.v(lambda e: e.reciprocal(out=r["gw"][:], in_=r["gsum"][:]), [r["gsum"]], [r["gw"]])
    P.v(lambda e: e.tensor_scalar(out=r["pen"][:], in0=r["gmask"][:], scalar1=-1.0, scalar2=BIG, op0=ALU.add,
                                  op1=ALU.mult), [r["gmask"]], [r["pen"]])
    P.v(lambda e: e.tensor_tensor(out=r["elm"][:].rearrange("p (g k) -> p g k", k=4),
                                  in0=L[:, 4:20].rearrange("p (g k) -> p g k", k=4),
                                  in1=r["pen"][:].unsqueeze(2).to_broadcast([128, 4, 4]), op=ALU.add),
        [L, r["pen"]], [r["elm"]])
    P.v(lambda e: e.tensor_reduce(out=r["m1"][:], in_=r["elm"][:], axis=AX.X, op=ALU.max), [r["elm"]], [r["m1"]])
    P.v(lambda e: e.tensor_scalar(out=r["mask1"][:], in0=r["elm"][:], scalar1=r["m1"][:, 0:1], scalar2=None,
                                  op0=ALU.is_ge), [r["elm"], r["m1"]], [r["mask1"]])
    P.v(lambda e: e.scalar_tensor_tensor(out=r["elm2"][:], in0=r["mask1"][:], scalar=-BIG, in1=r["elm"][:],
                                         op0=ALU.mult, op1=ALU.add), [r["mask1"], r["elm"]], [r["elm2"]])
    P.v(lambda e: e.tensor_reduce(out=r["m2"][:], in_=r["elm2"][:], axis=AX.X, op=ALU.max), [r["elm2"]], [r["m2"]])
    P.v(lambda e: e.tensor_scalar(out=r["mask2"][:], in0=r["elm2"][:], scalar1=r["m2"][:, 0:1], scalar2=None,
                                  op0=ALU.is_ge), [r["elm2"], r["m2"]], [r["mask2"]])
    P.v(lambda e: e.tensor_tensor(out=r["d"][:], in0=r["m1"][:], in1=r["m2"][:], op=ALU.subtract),
        [r["m1"], r["m2"]], [r["d"]])
    P.s(lambda e: e.activation(out=r["w1"][:], in_=r["d"][:], func=AF.Sigmoid), [r["d"]], [r["w1"]])
    P.s(lambda e: e.activation(out=r["w2"][:], in_=r["d"][:], func=AF.Sigmoid, scale=-1.0), [r["d"]], [r["w2"]])
    P.v(lambda e: e.tensor_tensor(out=r["w1"][:], in0=r["w1"][:], in1=r["gw"][:], op=ALU.mult),
        [r["w1"], r["gw"]], [r["w1"]])
    P.v(lambda e: e.tensor_tensor(out=r["w2"][:], in0=r["w2"][:], in1=r["gw"][:], op=ALU.mult),
        [r["w2"], r["gw"]], [r["w2"]])
    P.v(lambda e: e.tensor_scalar(out=r["comb"][:], in0=r["mask1"][:], scalar1=r["w1"][:, 0:1], scalar2=None,
                                  op0=ALU.mult), [r["mask1"], r["w1"]], [r["comb"]])
    P.v(lambda e: e.scalar_tensor_tensor(out=r["comb"][:], in0=r["mask2"][:], scalar=r["w2"][:, 0:1],
                                         in1=r["comb"][:], op0=ALU.mult, op1=ALU.add),
        [r["mask2"], r["w2"], r["comb"]], [r["comb"]])
    P.op("tensor", lambda e: e.transpose(out=pl[0:16, 128:256], in_=r["comb"][:], identity=C.ident[:]),
         [r["comb"], C.ident], [pl])
    P.v(lambda e: e.tensor_copy(out=R.combT[:, coff:coff + 128], in_=pl[0:16, 128:256]), [pl], [R.combT])


def emit_post_pass(P, C, R, MV, subs, x_src, y_src, x_dst):
    P.fence([R.act], R.p0)
    for si, (off, n, wh) in enumerate(subs):
        xt, xap = x_src(si)
        P.dma(R.xp[:, :, off:off + n], xap, [xt], [R.xp])
        yt, yap = y_src(si)
        yb = R.ybf[si % 2]
        P.dma(yb[:, :, 0:n], yap, [yt], [yb], eng="gpsimd")
        g1t, g1ap = MV.get(wh, "g1")
        for dc in range(8):
            pw = R.pw[dc % 2]
            for k in range(8):
                P.mm(pw[:, 0:n], R.wout[:, k, dc * 128:(dc + 1) * 128], yb[:, k, 0:n], [R.wout, yb], [pw],
                     start=(k == 0), stop=(k == 7))
            P.v(lambda e, dc=dc, pw=pw, off=off, n=n, g1ap=g1ap: e.scalar_tensor_tensor(
                out=R.xp[:, dc, off:off + n], in0=pw[:, 0:n], scalar=g1ap[:, dc:dc + 1], in1=R.xp[:, dc, off:off + n],
                op0=ALU.mult, op1=ALU.add), [pw, g1t, R.xp], [R.xp])
        emit_norm_mod(P, C, R.xp, off, n, MV.get(wh, "gm2"), MV.get(wh, "sh2"), R.pn, R.sq, R.rstd, R.tmp,
                      out_f32=R.hf, out_bf=None, ooff=0)
        for dc in range(8):
            P.v(lambda e, dc=dc, off=off, n=n: e.tensor_copy(out=R.hb[:, dc, off:off + n], in_=R.hf[:, dc, 0:n]),
                [R.hf], [R.hb])
        for tb in range(n // 128):
            emit_router_block(P, C, R, R.hf, tb * 128, off + tb * 128)
    P.fence(R.p0, [R.act])
    for ex in range(16):
        w = R.wgu[ex % 2]
        P.dma(w[:, 0, :, :], fm(R.d_wg[ex]), [R.d_wg], [w], eng="gpsimd")
        P.dma(w[:, 1, :, :], fm(R.d_wu[ex]), [R.d_wu], [w], eng="gpsimd")
        for si, (off, n, wh) in enumerate(subs):
            P.mm(R.pcb[:, 0:n], C.sel[:, ex, :], R.combT[:, off:off + n], [C.sel, R.combT], [R.pcb])
            for fc in range(2):
                i2 = (si * 2 + fc) % 2
                pg, pu, t1, t2 = R.pg[i2], R.pu[i2], R.t1[i2], R.t2[i2]
                for k in range(8):
                    P.mm(pg[:, 0:n], w[:, 0, k, fc * 128:(fc + 1) * 128], R.hb[:, k, off:off + n], [w, R.hb], [pg],
                         start=(k == 0), stop=(k == 7))
                for k in range(8):
                    P.mm(pu[:, 0:n], w[:, 1, k, fc * 128:(fc + 1) * 128], R.hb[:, k, off:off + n], [w, R.hb], [pu],
                         start=(k == 0), stop=(k == 7))
                P.s(lambda e, pg=pg, t1=t1, n=n: e.activation(out=t1[:, 0:n], in_=pg[:, 0:n], func=AF.Silu),
                    [pg], [t1])
                P.v(lambda e, pu=pu, t1=t1, t2=t2, n=n: e.tensor_tensor(out=t2[:, 0:n], in0=pu[:, 0:n], in1=t1[:, 0:n],
                                                                        op=ALU.mult), [pu, t1], [t2])
                P.v(lambda e, t2=t2, n=n, off=off, j=ex * 2 + fc: e.tensor_tensor(
                    out=R.act[:, j, off:off + n], in0=t2[:, 0:n], in1=R.pcb[:, 0:n], op=ALU.mult),
                    [t2, R.pcb], [R.act])
    for dc in range(8):
        w = R.wd[dc % 2]
        P.dma(w[:], R.d_wd[:].rearrange("e (c p) d -> p (e c) d", p=128)[:, :, dc * 128:(dc + 1) * 128],
              [R.d_wd], [w], eng="gpsimd")
        for si, (off, n, wh) in enumerate(subs):
            g2t, g2ap = MV.get(wh, "g2")
            pw = R.pw[(dc * len(subs) + si) % 2]
            for j in range(32):
                P.mm(pw[:, 0:n], w[:, j, :], R.act[:, j, off:off + n], [w, R.act], [pw], start=(j == 0), stop=(j == 31))
            P.v(lambda e, dc=dc, pw=pw, off=off, n=n, g2ap=g2ap: e.scalar_tensor_tensor(
                out=R.xp[:, dc, off:off + n], in0=pw[:, 0:n], scalar=g2ap[:, dc:dc + 1], in1=R.xp[:, dc, off:off + n],
                op0=ALU.mult, op1=ALU.add), [pw, g2t, R.xp], [R.xp])
    for si, (off, n, wh) in enumerate(subs):
        xt, xap = x_dst(si)
        P.dma(xap, R.xp[:, :, off:off + n], [R.xp], [xt])


def make_passes(n_lat, n_ctx):
    lat = [("L", i, min(512, n_lat - i)) for i in range(0, n_lat, 512)]
    h = len(lat) // 2
    p0, p1 = lat[:h], lat[h:]
    if n_ctx:
        c = n_ctx // 2
        p0 = p0 + [("C", n_lat, c)]
        p1 = p1 + [("C", n_lat + c, n_ctx - c)]
    return [p0, p1]


def build_post(n_lat, n_ctx):
    P = Prog()
    C = Consts(P)
    T = n_lat + n_ctx
    xT = P.din("xT", [1024, T])
    yT = P.din("yT", [1024, T])
    oT = P.dout("oT", [1024, T])
    MV = ModVecs(P, None, None)
    passes = make_passes(n_lat, n_ctx)
    PT = max(sum(t[2] for t in p) for p in passes)
    R = PostRes(P, C, PT)
    for p in passes:
        subs, srcs, off = [], [], 0
        for (wh, g0, n) in p:
            subs.append((off, n, wh))
            srcs.append(g0)
            off += n
        emit_post_pass(
            P, C, R, MV, subs,
            lambda si, srcs=srcs, subs=subs: (xT, fm(xT[:])[:, :, srcs[si]:srcs[si] + subs[si][1]]),
            lambda si, srcs=srcs, subs=subs: (yT, fm(yT[:])[:, :, srcs[si]:srcs[si] + subs[si][1]]),
            lambda si, srcs=srcs, subs=subs: (oT, fm(oT[:])[:, :, srcs[si]:srcs[si] + subs[si][1]]))
    return P.emit()


_CACHE = {}


def get_prog(key, builder, *args):
    if key not in _CACHE:
        _CACHE[key] = builder(*args)
    return _CACHE[key]


def run(nc, in_maps):
    res = run_bass_kernel_spmd(nc, in_maps, core_ids=list(range(NCORES)))
    return res.results


def to_fm_cols(a):
    a = np.asarray(a, np.float32)
    return np.ascontiguousarray(a.reshape(-1, 128).T)


def compute_mods(c, c_ctx, ada_w, ada_b):
    nc = get_prog("mod", build_mod)
    cvT = np.ascontiguousarray(np.concatenate([c, c_ctx[None, :]], 0).T)
    wall = np.concatenate([ada_w[l] for l in range(DEPTH)], axis=1)
    ball = np.concatenate([ada_b[l] for l in range(DEPTH)], axis=0)
    in_maps = []
    for k in range(NCORES):
        cols = slice(k * 3072, (k + 1) * 3072)
        in_maps.append({"cvT": cvT, "w": np.ascontiguousarray(wall[:, cols]),
                        "b": to_fm_cols(ball[cols])})
    res = run(nc, in_maps)
    full = np.concatenate([r["out"] for r in res], axis=1)
    return np.ascontiguousarray(full.reshape(128, DEPTH, 48, 5).transpose(1, 0, 2, 3))


WROWS = 40
WTOK = WROWS * 64
OWN0 = 256
NOWN = 2048


def attn_row_tiles(j):
    if j == 4:
        return [0, 2, 4, 6, 8, 10]
    if j == 5:
        return [1, 3, 5, 7, 9, 11]
    if j == 6:
        return [2, 4, 6, 8, 10]
    if j == 7:
        return [3, 5, 7, 9, 11]
    if j == 33:
        return [28, 30, 32, 34, 36]
    if j == 34:
        return [28, 30, 32, 34, 36]
    if j == 35:
        return [28, 30, 32, 34, 36, 38]
    return [j - 4, j - 2, j, j + 2]


EDGE_ROWS = (4, 5, 6, 7, 33, 34, 35)


def edge_tile_index():
    idx, n = {}, 0
    for j in EDGE_ROWS:
        for t in attn_row_tiles(j):
            idx[(j, t)] = n
            n += 1
    return idx, n


def even_host_tables(half, rpb):
    kc = np.arange(64)[:, None]
    qc = np.arange(64)[None, :]
    cidx = np.clip(kc - qc, -15, 15) + 15
    cstart = np.clip(qc - 8, 0, 48)
    cvalid = ((kc >= cstart) & (kc < cstart + 16)).astype(np.float32)
    eb = np.zeros((128, 8, 16, 64), np.float32)
    for s in range(16):
        r0 = min(s, 14)
        r1 = min(s + 1, 14)
        eb[0:64, :, s, :] = rpb[:, r0][:, cidx].transpose(1, 0, 2)
        eb[64:128, :, s, :] = rpb[:, r1][:, cidx].transpose(1, 0, 2)
    cmask = np.concatenate([cvalid, cvalid], 0)
    idx, n = edge_tile_index()
    rm = np.zeros((128, n), np.float32)
    for (j, t), i in idx.items():
        R = j - 4 + 32 * half
        gs = min(max(R - 4, 0), 56)
        for k, lrow in enumerate((t, t + 1)):
            grow = lrow - 4 + 32 * half
            rm[64 * k:64 * (k + 1), i] = 1.0 if (gs <= grow < gs + 8) else 0.0
    edge = np.zeros((128, 2), np.float32)
    edge[:, 0] = 1.0 if half == 1 else 0.0
    edge[:, 1] = 1.0 if half == 0 else 0.0
    return {"ebraw": eb, "cmask": cmask, "rowmask": rm, "edge": edge}


def build_even_mixer(with_ctx_q):
    P = Prog()
    C = Consts(P, need_sel=False)
    NCTX = CTX_LEN
    NTOK = WTOK + NCTX
    xw = P.din("xwT", [1024, WTOK])
    cx = P.din("ctxT", [1024, NCTX])
    w_in = P.din("w_in", [1024, 3072])
    d_qg = P.din("qg", [128, 1])
    d_kg = P.din("kg", [128, 1])
    d_eb = P.din("ebraw", [128, 8, 16, 64])
    d_cm = P.din("cmask", [128, 64])
    idx_edge, n_edge = edge_tile_index()
    d_rm = P.din("rowmask", [128, n_edge])
    d_edge = P.din("edge", [128, 2])
    d_scw = P.din("scw", [128, 4, 3])
    o_att = P.dout("y_att", [NOWN, 512])
    o_sc = P.dout("y_scT", [512, NOWN])
    if with_ctx_q:
        o_attc = P.dout("y_att_c", [NCTX, 512])
        o_scc = P.dout("y_scT_c", [512, NCTX])
    MV = ModVecs(P, None, None)

    u = P.sb("u", [128, 8, NTOK], BF16)
    nq = NOWN + (NCTX if with_ctx_q else 0)
    qT = P.sb("qT", [128, 4, nq], BF16)
    kT = P.sb("kT", [128, 4, NTOK], BF16)
    NBE = WTOK // 128 + 2
    NBO = WTOK // 128 - 1
    Ve = P.sb("Ve", [128, NBE, 8, 65], BF16)
    Vo = P.sb("Vo", [128, NBO, 8, 65], BF16)
    wb = [P.sb(f"wb{i}", [128, 8, 512], BF16) for i in range(2)]
    rstd = P.sb("rstd", [128, 512])
    qg = P.sb("qg_s", [128, 1])
    kg = P.sb("kg_s", [128, 1])
    cm = P.sb("cm_s", [128, 64])
    rm = P.sb("rm_s", [128, n_edge])
    edge = P.sb("edge_s", [128, 2])
    scw = P.sb("scw_s", [128, 4, 3])
    blk = P.sb("blk", [128, 128], BF16)
    eps_q = P.sb("eps_q", [128, 1])
    for t, d in ((qg, d_qg), (kg, d_kg), (cm, d_cm), (rm, d_rm), (edge, d_edge), (scw, d_scw)):
        P.dma(t[:], d[:], [d], [t])
    P.v(lambda e: e.memset(blk[:], 0.0), [], [blk])
    P.v(lambda e: e.memset(blk[0:64, 0:64], 1.0 / 64), [], [blk])
    P.v(lambda e: e.memset(blk[64:128, 64:128], 1.0 / 64), [], [blk])
    P.v(lambda e: e.memset(eps_q[:], RMS_EPS), [], [eps_q])
    P.v(lambda e: e.memset(Ve[:, :, :, 64:65], 1.0), [], [Ve])
    P.v(lambda e: e.memset(Vo[:, :, :, 64:65], 1.0), [], [Vo])

    S = P.nc.alloc_sbuf_tensor("scr", [128, 7168], F32)
    xs = P.view("xs", S[:, 0:4096].rearrange("p (c n) -> p c n", c=8))
    sq = P.view("sq", S[:, 4096:6144].bitcast(BF16).rearrange("p (c n) -> p c n", c=8))
    tmp = [P.view(f"tmpn{i}", S[:, 6144 + 512 * i:6656 + 512 * i]) for i in range(2)]
    EB = P.view("EB", S[:, 0:4096].bitcast(BF16).rearrange("p (h s q) -> p h s q", h=8, s=16))
    ebs = P.view("ebs", S[:, 4096:5120].rearrange("p (s q) -> p s q", s=16))
    pT = [P.view(f"pT{i}", S[:, 5120 + 256 * i:5376 + 256 * i].bitcast(BF16)) for i in range(2)]
    yrow = [P.view(f"yrow{i}", S[0:64, 5632 + 512 * i:6144 + 512 * i]) for i in range(2)]
    later = [EB, ebs] + pT + yrow
    pn = P.ps("pn")
    pq = [P.ps(f"pq{i}") for i in range(2)]
    pss = [P.ps(f"pss{i}") for i in range(2)]
    po = [P.ps(f"po{i}") for i in range(2)]
    pm = P.ps("pm")

    subsN = [("L", xw, c0, min(512, WTOK - c0), c0) for c0 in range(0, WTOK, 512)] + [("C", cx, 0, NCTX, WTOK)]
    for (wh, src, c0, n, uoff) in subsN:
        P.dma(xs[:, :, 0:n], fm(src[:])[:, :, c0:c0 + n], [src], [xs])
        emit_norm_mod(P, C, xs, 0, n, MV.get(wh, "gm1"), MV.get(wh, "sh1"), pn, sq, rstd, tmp,
                      out_f32=None, out_bf=u, ooff=uoff)
    P.fence([xs, sq] + tmp, later)

    for h in range(8):
        P.dma(ebs[:], d_eb[:, h, :, :], [d_eb], [ebs])
        P.s(lambda e: e.activation(out=ebs[:], in_=ebs[:], func=AF.Exp), [ebs], [ebs])
        P.v(lambda e, h=h: e.tensor_tensor(out=EB[:, h, :, :], in0=ebs[:],
                                           in1=cm[:].unsqueeze(1).to_broadcast([128, 16, 64]), op=ALU.mult),
            [ebs, cm], [EB])

    def qk_phase(col0, gain, dst, ranges, wi):
        w = wb[wi]
        P.dma(w[:], fm(w_in[:])[:, :, col0:col0 + 512], [w_in], [w], eng="gpsimd")
        it = 0
        for (uoff, n, doff) in ranges:
            for ch in range(4):
                p = pq[it % 2]
                it += 1
                for k in range(8):
                    P.mm(p[:, 0:n], w[:, k, ch * 128:(ch + 1) * 128], u[:, k, uoff:uoff + n], [w, u], [p],
                         start=(k == 0), stop=(k == 7))
                P.s(lambda e, p=p, n=n: e.activation(out=qsq[:, 0:n], in_=p[:, 0:n], func=AF.Square), [p], [qsq])
                P.mm(pm[:, 0:n], blk[:], qsq[:, 0:n], [blk, qsq], [pm])
                P.s(lambda e, n=n: e.activation(out=rstd[:, 0:n], in_=pm[:, 0:n], func=AF.Sqrt, bias=eps_q[:, 0:1],
                                                scale=1.0), [pm, eps_q], [rstd])
                P.v(lambda e, n=n: e.reciprocal(out=rstd[:, 0:n], in_=rstd[:, 0:n]), [rstd], [rstd])
                P.v(lambda e, p=p, n=n: e.tensor_tensor(out=qtmp[:, 0:n], in0=p[:, 0:n], in1=rstd[:, 0:n],
                                                        op=ALU.mult), [p, rstd], [qtmp])
                P.s(lambda e, n=n, ch=ch, doff=doff: e.activation(out=dst[:, ch, doff:doff + n], in_=qtmp[:, 0:n],
                                                                  func=AF.Copy, scale=gain[:, 0:1]),
                    [qtmp, gain], [dst])

    qsq = P.sb("qsq", [128, 512], BF16)
    qtmp = P.sb("qtmp", [128, 512])
    q_ranges = [(OWN0 + c0, 512, c0) for c0 in range(0, NOWN, 512)]
    if with_ctx_q:
        q_ranges.append((WTOK, NCTX, NOWN))
    k_ranges = [(c0, 512, c0) for c0 in range(0, WTOK, 512)] + [(WTOK, NCTX, WTOK)]
    qk_phase(0, qg, qT, q_ranges, 0)
    qk_phase(512, kg, kT, k_ranges, 1)

    w = wb[0]
    P.dma(w[:], fm(w_in[:])[:, :, 1024:1536], [w_in], [w], eng="gpsimd")
    vblocks = [(Ve, b, 128 * b) for b in range(WTOK // 128)] + [(Ve, WTOK // 128 + b, WTOK + 128 * b) for b in range(2)] \
        + [(Vo, b, 64 + 128 * b) for b in range(NBO)]
    for it, (Vt, b, tok) in enumerate(vblocks):
        p = pq[it % 2]
        for k in range(8):
            P.mm(p[:, 0:512], u[:, k, tok:tok + 128], w[:, k, :], [u, w], [p], start=(k == 0), stop=(k == 7))
        fnc = (lambda e, Vt=Vt, b=b, p=p: e.activation(out=Vt[:, b, :, 0:64],
                                                       in_=p[:, 0:512].rearrange("p (h d) -> p h d", h=8),
                                                       func=AF.Copy))
        if it % 2 == 0:
            P.s(fnc, [p], [Vt])
        else:
            P.v(lambda e, Vt=Vt, b=b, p=p: e.tensor_copy(out=Vt[:, b, :, 0:64],
                                                         in_=p[:, 0:512].rearrange("p (h d) -> p h d", h=8)),
                [p], [Vt])

    def attend(qoff, tiles, out_t, out_ap_fn, gi):
        yr = yrow[gi % 2]
        nt = len(tiles)
        for h in range(8):
            hp, hc = (h % 2) * 64, h // 2
            ps_ = pss[h % 2]
            pt = pT[h % 2]
            for i, (ktok, Vt, vb, slot, rmi) in enumerate(tiles):
                P.mm(ps_[:, i * 64:(i + 1) * 64], kT[hp:hp + 64, hc, ktok:ktok + 128],
                     qT[hp:hp + 64, hc, qoff:qoff + 64], [kT, qT], [ps_])
            P.s(lambda e, ps_=ps_, pt=pt, nt=nt: e.activation(out=pt[:, 0:nt * 64], in_=ps_[:, 0:nt * 64],
                                                              func=AF.Exp, scale=0.125), [ps_], [pt])
            nb = sum(1 for t in tiles if t[3] is not None)
            if nb:
                s0 = tiles[0][3]
                P.v(lambda e, pt=pt, nb=nb, s0=s0, h=h: e.tensor_tensor(
                    out=pt[:, 0:nb * 64].rearrange("p (t q) -> p t q", t=nb),
                    in0=pt[:, 0:nb * 64].rearrange("p (t q) -> p t q", t=nb),
                    in1=EB[:, h, s0:s0 + 2 * nb - 1:2, :], op=ALU.mult), [pt, EB], [pt])
            for i, (ktok, Vt, vb, slot, rmi) in enumerate(tiles):
                if rmi is not None:
                    P.v(lambda e, pt=pt, i=i, rmi=rmi: e.tensor_scalar(
                        out=pt[:, i * 64:(i + 1) * 64], in0=pt[:, i * 64:(i + 1) * 64], scalar1=rm[:, rmi:rmi + 1],
                        scalar2=None, op0=ALU.mult), [pt, rm], [pt])
            po_ = po[h % 2]
            for i, (ktok, Vt, vb, slot, rmi) in enumerate(tiles):
                P.mm(po_[0:64, 0:65], pt[:, i * 64:(i + 1) * 64], Vt[:, vb, h, :], [pt, Vt], [po_],
                     start=(i == 0), stop=(i == nt - 1))
            P.v(lambda e, po_=po_: e.reciprocal(out=rs[0:64, :], in_=po_[0:64, 64:65]), [po_], [rs])
            P.v(lambda e, po_=po_, yr=yr, h=h: e.tensor_scalar(out=yr[:, h * 64:(h + 1) * 64], in0=po_[0:64, 0:64],
                                                               scalar1=rs[0:64, 0:1], scalar2=None, op0=ALU.mult),
                [po_, rs], [yr])
        P.dma(out_ap_fn(), yr[:, :], [yr], [out_t])

    rs = P.sb("rs", [128, 1])
    ctx_tiles = [(WTOK + 128 * b, Ve, WTOK // 128 + b, None, None) for b in range(2)]
    gi = 0
    for j in range(4, 36):
        tiles = []
        for t in attn_row_tiles(j):
            Vt, vb = (Ve, t // 2) if t % 2 == 0 else (Vo, (t - 1) // 2)
            tiles.append((t * 64, Vt, vb, t - j + 7, idx_edge.get((j, t))))
        tiles += ctx_tiles
        attend((j - 4) * 64, tiles, o_att, lambda j=j: o_att[(j - 4) * 64:(j - 3) * 64, :], gi)
        gi += 1
    if with_ctx_q:
        for g in range(NCTX // 64):
            attend(NOWN + g * 64, ctx_tiles, o_attc, lambda g=g: o_attc[g * 64:(g + 1) * 64, :], gi)
            gi += 1

    wS = [P.sb(f"wS{i}", [128, 8, 512], BF16) for i in range(1)]
    P.dma(wb[0][:], fm(w_in[:])[:, :, 1536:2048], [w_in], [wb[0]], eng="gpsimd")
    P.dma(wb[1][:], fm(w_in[:])[:, :, 2048:2560], [w_in], [wb[1]], eng="gpsimd")
    P.dma(wS[0][:], fm(w_in[:])[:, :, 2560:3072], [w_in], [wS[0]], eng="gpsimd")
    gcs = P.sb("gcs", [128, 512])
    pp = P.sb("pp", [128, 512])
    oo = P.sb("oo", [128, 512])
    ysc = [P.sb(f"ysc{i}", [128, 512]) for i in range(2)]
    sc_jobs = []
    for t0 in range(0, NOWN, 510):
        n = min(510, NOWN - t0)
        sc_jobs.append((OWN0 + t0 - 1, n, o_sc, t0, "lat", t0 == 0, t0 + n == NOWN))
    if with_ctx_q:
        sc_jobs.append((WTOK - 1, NCTX, o_scc, 0, "ctx", True, True))
    it = 0
    for (c0, n, dst, d0, kind, first, last) in sc_jobs:
        lo, hi = (1, n + 1) if kind == "ctx" else (0, n + 2)
        for ch in range(4):
            pg_, ph_ = pq[0], pq[1]
            for k in range(8):
                P.mm(pg_[:, lo:hi], wb[1][:, k, ch * 128:(ch + 1) * 128], u[:, k, c0 + lo:c0 + hi], [wb[1], u], [pg_],
                     start=(k == 0), stop=(k == 7))
            for k in range(8):
                P.mm(ph_[:, lo:hi], wS[0][:, k, ch * 128:(ch + 1) * 128], u[:, k, c0 + lo:c0 + hi], [wS[0], u], [ph_],
                     start=(k == 0), stop=(k == 7))
            P.s(lambda e, pg_=pg_, lo=lo, hi=hi: e.activation(out=gcs[:, lo:hi], in_=pg_[:, lo:hi], func=AF.Copy),
                [pg_], [gcs])
            P.v(lambda e, ph_=ph_, lo=lo, hi=hi: e.tensor_tensor(out=pp[:, lo:hi], in0=ph_[:, lo:hi], in1=gcs[:, lo:hi],
                                                                 op=ALU.mult), [ph_, gcs], [pp])
            if kind == "ctx":
                P.v(lambda e: e.memset(pp[:, 0:1], 0.0), [], [pp])
                P.v(lambda e, n=n: e.memset(pp[:, n + 1:n + 2], 0.0), [], [pp])
            else:
                if first:
                    P.v(lambda e: e.tensor_scalar(out=pp[:, 0:1], in0=pp[:, 0:1], scalar1=edge[:, 0:1], scalar2=None,
                                                  op0=ALU.mult), [pp, edge], [pp])
                if last:
                    P.v(lambda e, n=n: e.tensor_scalar(out=pp[:, n + 1:n + 2], in0=pp[:, n + 1:n + 2],
                                                       scalar1=edge[:, 1:2], scalar2=None, op0=ALU.mult),
                        [pp, edge], [pp])
            P.v(lambda e, n=n, ch=ch: e.tensor_scalar(out=oo[:, 0:n], in0=pp[:, 1:n + 1], scalar1=scw[:, ch, 1:2],
                                                      scalar2=None, op0=ALU.mult), [pp, scw], [oo])
            P.v(lambda e, n=n, ch=ch: e.scalar_tensor_tensor(out=oo[:, 0:n], in0=pp[:, 0:n], scalar=scw[:, ch, 0:1],
                                                             in1=oo[:, 0:n], op0=ALU.mult, op1=ALU.add),
                [pp, scw, oo], [oo])
            P.v(lambda e, n=n, ch=ch: e.scalar_tensor_tensor(out=oo[:, 0:n], in0=pp[:, 2:n + 2], scalar=scw[:, ch, 2:3],
                                                             in1=oo[:, 0:n], op0=ALU.mult, op1=ALU.add),
                [pp, scw, oo], [oo])
            pb_ = pm
            for k in range(8):
                P.mm(pb_[:, 0:n], wb[0][:, k, ch * 128:(ch + 1) * 128], u[:, k, c0 + 1:c0 + 1 + n], [wb[0], u], [pb_],
                     start=(k == 0), stop=(k == 7))
            yt = ysc[it % 2]
            it += 1
            P.v(lambda e, pb_=pb_, yt=yt, n=n: e.tensor_tensor(out=yt[:, 0:n], in0=pb_[:, 0:n], in1=oo[:, 0:n],
                                                               op=ALU.mult), [pb_, oo], [yt])
            P.dma(dst[ch * 128:(ch + 1) * 128, d0:d0 + n], yt[:, 0:n], [yt], [dst])
    return P.emit()


def even_mixer_inputs(l, b, half, x, ctx, modT, p):
    e = l // 2
    g0 = (32 * half - 4) * 64
    xw = np.zeros((WTOK, D_MODEL), np.float32)
    lo, hi = max(g0, 0), min(g0 + WTOK, SEQ)
    xw[lo - g0:hi - g0] = x[b, lo:hi]
    m = dict(c_ident=np.eye(128, dtype=np.float32))
    m.update(even_host_tables(half, p["na_rpb"][e]))
    m.update(xwT=np.ascontiguousarray(xw.T), ctxT=np.ascontiguousarray(ctx[b].T),
             modL=np.ascontiguousarray(modT[l, :, :, b]), modC=np.ascontiguousarray(modT[l, :, :, 4]),
             n1g=to_fm_cols(p["norm1_g"][l]), n2g=to_fm_cols(p["norm2_g"][l]),
             w_in=p["w_in_even"][e],
             qg=np.ascontiguousarray(np.tile(p["qn_g"][e], 2)[:, None]),
             kg=np.ascontiguousarray(np.tile(p["kn_g"][e], 2)[:, None]),
             scw=np.ascontiguousarray(p["sc_conv_w"][e].reshape(3, 4, 128).transpose(2, 1, 0)))
    return m


OH = 16
OWTOK = NOWN + 2 * OH


def build_odd_a(with_ctx):
    P = Prog()
    C = Consts(P, need_sel=False)
    NCTX = CTX_LEN if with_ctx else 0
    NTOK = OWTOK + NCTX
    xw = P.din("xwT", [1024, OWTOK])
    if with_ctx:
        cx = P.din("ctxT", [1024, CTX_LEN])
    w_in = P.din("w_in", [1024, 2560])
    d_edge = P.din("edge", [128, 2])
    d_hsw = P.din("hsw", [128, 12, 3])
    d_cfw = P.din("cfw", [128, 4, 31])
    d_cfb = P.din("cfb", [128, 4])
    d_lng = P.din("lng", [128, 4])
    d_lnb = P.din("lnb", [128, 4])
    o_hy = P.dout("hyT", [1536, NOWN + NCTX])
    o_cf = P.dout("cfT", [512, NOWN + NCTX])
    MV = ModVecs(P, None, None)
    u = P.sb("u", [128, 8, NTOK], BF16)
    w = P.sb("w", [128, 8, 2560], BF16)
    for i in range(5):
        P.dma(w[:, :, i * 512:(i + 1) * 512], fm(w_in[:])[:, :, i * 512:(i + 1) * 512], [w_in], [w], eng="gpsimd")
    edge = P.sb("edge_s", [128, 2])
    hsw = P.sb("hsw_s", [128, 12, 3])
    cfw = P.sb("cfw_s", [128, 4, 31])
    cfb = P.sb("cfb_s", [128, 4])
    lng = P.sb("lng_s", [128, 4])
    lnb = P.sb("lnb_s", [128, 4])
    for t, d in ((edge, d_edge), (hsw, d_hsw), (cfw, d_cfw), (cfb, d_cfb), (lng, d_lng), (lnb, d_lnb)):
        P.dma(t[:], d[:], [d], [t])
    ones_ln = P.sb("ones_ln", [128, 128])
    eps_ln = P.sb("eps_ln", [128, 1])
    P.v(lambda e: e.memset(ones_ln[:], 1.0 / 512), [], [ones_ln])
    P.v(lambda e: e.memset(eps_ln[:], LN_EPS), [], [eps_ln])
    xs = P.sb("xs", [128, 8, 512])
    sq = P.sb("sq", [128, 8, 512], BF16)
    tmp = [P.sb(f"tmpn{i}", [128, 512]) for i in range(2)]
    rstd = P.sb("rstd", [128, 512])
    pn = P.ps("pn")
    pq = [P.ps(f"pq{i}") for i in range(4)]
    pmean = P.ps("pmean")
    pex2 = P.ps("pex2")

    subsN = [("L", xw, c0, min(512, OWTOK - c0), c0) for c0 in range(0, OWTOK, 512)]
    if with_ctx:
        subsN.append(("C", cx, 0, CTX_LEN, OWTOK))
    for (wh, src, c0, n, uoff) in subsN:
        P.dma(xs[:, :, 0:n], fm(src[:])[:, :, c0:c0 + n], [src], [xs])
        emit_norm_mod(P, C, xs, 0, n, MV.get(wh, "gm1"), MV.get(wh, "sh1"), pn, sq, rstd, tmp,
                      out_f32=None, out_bf=u, ooff=uoff)

    hp = [P.sb(f"hp{i}", [128, 512]) for i in range(2)]
    ho = [P.sb(f"ho{i}", [128, 512]) for i in range(2)]
    jobs = []
    for t0 in range(0, NOWN, 510):
        n = min(510, NOWN - t0)
        jobs.append((OH + t0 - 1, n, t0, "lat", t0 == 0, t0 + n == NOWN))
    if with_ctx:
        jobs.append((OWTOK - 1, CTX_LEN, NOWN, "ctx", True, True))
    it = 0
    for (c0, n, d0, kind, first, last) in jobs:
        lo, hi = (1, n + 1) if kind == "ctx" else (0, n + 2)
        for ch in range(12):
            p = pq[it % 4]
            pp, oo = hp[it % 2], ho[it % 2]
            it += 1
            for k in range(8):
                P.mm(p[:, lo:hi], w[:, k, ch * 128:(ch + 1) * 128], u[:, k, c0 + lo:c0 + hi], [w, u], [p],
                     start=(k == 0), stop=(k == 7))
            P.s(lambda e, p=p, pp=pp, lo=lo, hi=hi: e.activation(out=pp[:, lo:hi], in_=p[:, lo:hi], func=AF.Copy),
                [p], [pp])
            if kind == "ctx":
                P.v(lambda e, pp=pp: e.memset(pp[:, 0:1], 0.0), [], [pp])
                P.v(lambda e, pp=pp, n=n: e.memset(pp[:, n + 1:n + 2], 0.0), [], [pp])
            else:
                if first:
                    P.v(lambda e, pp=pp: e.tensor_scalar(out=pp[:, 0:1], in0=pp[:, 0:1], scalar1=edge[:, 0:1],
                                                         scalar2=None, op0=ALU.mult), [pp, edge], [pp])
                if last:
                    P.v(lambda e, pp=pp, n=n: e.tensor_scalar(out=pp[:, n + 1:n + 2], in0=pp[:, n + 1:n + 2],
                                                              scalar1=edge[:, 1:2], scalar2=None, op0=ALU.mult),
                        [pp, edge], [pp])
            P.v(lambda e, pp=pp, oo=oo, n=n, ch=ch: e.tensor_scalar(out=oo[:, 0:n], in0=pp[:, 1:n + 1],
                                                                    scalar1=hsw[:, ch, 1:2], scalar2=None,
                                                                    op0=ALU.mult), [pp, hsw], [oo])
            P.v(lambda e, pp=pp, oo=oo, n=n, ch=ch: e.scalar_tensor_tensor(
                out=oo[:, 0:n], in0=pp[:, 0:n], scalar=hsw[:, ch, 0:1], in1=oo[:, 0:n], op0=ALU.mult, op1=ALU.add),
                [pp, hsw, oo], [oo])
            P.v(lambda e, pp=pp, oo=oo, n=n, ch=ch: e.scalar_tensor_tensor(
                out=oo[:, 0:n], in0=pp[:, 2:n + 2], scalar=hsw[:, ch, 2:3], in1=oo[:, 0:n], op0=ALU.mult, op1=ALU.add),
                [pp, hsw, oo], [oo])
            P.dma(o_hy[ch * 128:(ch + 1) * 128, d0:d0 + n], oo[:, 0:n], [oo], [o_hy])

    NS = 482
    glu = P.sb("glu", [128, 4, 512])
    sig = P.sb("sig", [128, 512])
    cacc = P.sb("cacc", [128, 4, 512])
    cacc2 = P.sb("cacc2", [128, 4, 512])
    csq = P.sb("csq", [128, 512])
    mean = P.sb("mean", [128, 512])
    var = P.sb("var", [128, 512])
    cy = [P.sb(f"cy{i}", [128, 512]) for i in range(2)]
    cjobs = []
    for t0 in range(0, NOWN, NS):
        n = min(NS, NOWN - t0)
        cjobs.append((OH + t0 - 15, n, t0, "lat", t0 == 0, t0 + n == NOWN))
    if with_ctx:
        cjobs.append((OWTOK - 15, CTX_LEN, NOWN, "ctx", True, True))
    it = 0
    for (c0, n, d0, kind, first, last) in cjobs:
        lo, hi = (15, n + 15) if kind == "ctx" else (0, n + 30)
        for ch in range(4):
            pa, pg_ = pq[(2 * ch) % 4], pq[(2 * ch + 1) % 4]
            for k in range(8):
                P.mm(pa[:, lo:hi], w[:, k, 1536 + ch * 128:1536 + (ch + 1) * 128], u[:, k, c0 + lo:c0 + hi], [w, u], [pa],
                     start=(k == 0), stop=(k == 7))
            for k in range(8):
                P.mm(pg_[:, lo:hi], w[:, k, 2048 + ch * 128:2048 + (ch + 1) * 128], u[:, k, c0 + lo:c0 + hi], [w, u],
                     [pg_], start=(k == 0), stop=(k == 7))
            P.s(lambda e, pg_=pg_, lo=lo, hi=hi: e.activation(out=sig[:, lo:hi], in_=pg_[:, lo:hi], func=AF.Sigmoid),
                [pg_], [sig])
            P.v(lambda e, pa=pa, ch=ch, lo=lo, hi=hi: e.tensor_tensor(out=glu[:, ch, lo:hi], in0=pa[:, lo:hi],
                                                                      in1=sig[:, lo:hi], op=ALU.mult),
                [pa, sig], [glu])
            if kind == "ctx":
                P.v(lambda e, ch=ch: e.memset(glu[:, ch, 0:15], 0.0), [], [glu])
                P.v(lambda e, ch=ch, n=n: e.memset(glu[:, ch, n + 15:n + 30], 0.0), [], [glu])
            else:
                if first:
                    P.v(lambda e, ch=ch: e.tensor_scalar(out=glu[:, ch, 0:15], in0=glu[:, ch, 0:15],
                                                         scalar1=edge[:, 0:1], scalar2=None, op0=ALU.mult),
                        [glu, edge], [glu])
                if last:
                    P.v(lambda e, ch=ch, n=n: e.tensor_scalar(out=glu[:, ch, n + 15:n + 30], in0=glu[:, ch, n + 15:n + 30],
                                                              scalar1=edge[:, 1:2], scalar2=None, op0=ALU.mult),
                        [glu, edge], [glu])
        for ch in range(4):
            P.v(lambda e, ch=ch, n=n: e.tensor_scalar(out=cacc[:, ch, 0:n], in0=glu[:, ch, 0:n],
                                                      scalar1=cfw[:, ch, 0:1], scalar2=cfb[:, ch:ch + 1],
                                                      op0=ALU.mult, op1=ALU.add), [glu, cfw, cfb], [cacc])
            for j in range(1, 31):
                P.v(lambda e, ch=ch, n=n, j=j: e.scalar_tensor_tensor(
                    out=cacc[:, ch, 0:n], in0=glu[:, ch, j:j + n], scalar=cfw[:, ch, j:j + 1], in1=cacc[:, ch, 0:n],
                    op0=ALU.mult, op1=ALU.add), [glu, cfw, cacc], [cacc])
        for ch in range(4):
            P.mm(pmean[:, 0:n], ones_ln[:], cacc[:, ch, 0:n], [ones_ln, cacc], [pmean], start=(ch == 0), stop=(ch == 3))
        for ch in range(4):
            P.s(lambda e, ch=ch, n=n: e.activation(out=csq[:, 0:n], in_=cacc[:, ch, 0:n], func=AF.Square),
                [cacc], [csq])
            P.mm(pex2[:, 0:n], ones_ln[:], csq[:, 0:n], [ones_ln, csq], [pex2], start=(ch == 0), stop=(ch == 3))
        P.s(lambda e, n=n: e.activation(out=mean[:, 0:n], in_=pmean[:, 0:n], func=AF.Copy), [pmean], [mean])
        P.v(lambda e, n=n: e.tensor_tensor(out=var[:, 0:n], in0=mean[:, 0:n], in1=mean[:, 0:n], op=ALU.mult),
            [mean], [var])
        P.v(lambda e, n=n: e.tensor_tensor(out=var[:, 0:n], in0=pex2[:, 0:n], in1=var[:, 0:n], op=ALU.subtract),
            [pex2, var], [var])
        P.s(lambda e, n=n: e.activation(out=var[:, 0:n], in_=var[:, 0:n], func=AF.Sqrt, bias=eps_ln[:, 0:1], scale=1.0),
            [var, eps_ln], [var])
        P.v(lambda e, n=n: e.reciprocal(out=var[:, 0:n], in_=var[:, 0:n]), [var], [var])
        for ch in range(4):
            yt = cy[it % 2]
            it += 1
            P.v(lambda e, ch=ch, n=n: e.tensor_tensor(out=cacc[:, ch, 0:n], in0=cacc[:, ch, 0:n], in1=mean[:, 0:n],
                                                      op=ALU.subtract), [cacc, mean], [cacc])
            P.v(lambda e, ch=ch, n=n: e.tensor_tensor(out=cacc[:, ch, 0:n], in0=cacc[:, ch, 0:n], in1=var[:, 0:n],
                                                      op=ALU.mult), [cacc, var], [cacc])
            P.s(lambda e, ch=ch, n=n, yt=yt: e.activation(out=yt[:, 0:n], in_=cacc[:, ch, 0:n], func=AF.Silu,
                                                          scale=lng[:, ch:ch + 1], bias=lnb[:, ch:ch + 1]),
                [cacc, lng, lnb], [yt])
            P.dma(o_cf[ch * 128:(ch + 1) * 128, d0:d0 + n], yt[:, 0:n], [yt], [o_cf])
    return P.emit()


def odd_a_inputs(l, b, half, x, ctx, modT, p, with_ctx):
    o = l // 2
    g0 = half * NOWN - OH
    xw = np.zeros((OWTOK, D_MODEL), np.float32)
    lo, hi = max(g0, 0), min(g0 + OWTOK, SEQ)
    xw[lo - g0:hi - g0] = x[b, lo:hi]
    edge = np.zeros((128, 2), np.float32)
    edge[:, 0] = 1.0 if half == 1 else 0.0
    edge[:, 1] = 1.0 if half == 0 else 0.0
    m = dict(c_ident=np.eye(128, dtype=np.float32), edge=edge,
             xwT=np.ascontiguousarray(xw.T),
             modL=np.ascontiguousarray(modT[l, :, :, b]), modC=np.ascontiguousarray(modT[l, :, :, 4]),
             n1g=to_fm_cols(p["norm1_g"][l]), n2g=to_fm_cols(p["norm2_g"][l]),
             w_in=p["w_in_odd"][o],
             hsw=np.ascontiguousarray(p["hy_short_w"][o].reshape(3, 12, 128).transpose(2, 1, 0)),
             cfw=np.ascontiguousarray(p["cf_conv_w"][o].reshape(31, 4, 128).transpose(2, 1, 0)),
             cfb=to_fm_cols(p["cf_conv_b"][o]), lng=to_fm_cols(p["cf_ln_g"][o]), lnb=to_fm_cols(p["cf_ln_b"][o]))
    if with_ctx:
        m["ctxT"] = np.ascontiguousarray(ctx[b].T)
    return m


HY_W = 512
NK1 = 33
SBK = 64


def hyena_consts(L=SEQ):
    f32 = np.float32
    t = np.linspace(0.0, 1.0, L, dtype=f32)[:, None]
    w = (2.0 * math.pi * np.arange(L, dtype=f32)[:, None] / L).astype(f32)
    bands = np.linspace(1e-4, 15, 16, dtype=f32)[None, :]
    zf = np.concatenate([t, np.cos(bands * w), -np.sin(bands * w)], axis=-1).astype(f32)
    out = {"zfT": np.ascontiguousarray(zf.T)}
    n1 = np.arange(32)[:, None]
    k1 = np.arange(NK1)[None, :]
    th = 2 * np.pi * n1 * k1 / 64.0
    out["F1m"] = np.concatenate([np.cos(th), -np.sin(th)], 1).astype(f32)
    n2 = np.arange(128)[:, None, None]
    kk1 = np.arange(NK1)[None, :, None]
    k2 = np.arange(128)[None, None, :]
    ph = 2 * np.pi * (n2 * kk1 / 8192.0 + n2 * k2 / 128.0)
    M = np.stack([np.cos(ph), -np.sin(ph), np.sin(ph)], 2)
    out["Mtab"] = M.astype(f32)
    kq = np.arange(128)[:, None, None]
    mq = np.arange(128)[None, None, :]
    phg = 2 * np.pi * (mq * kk1 / 8192.0 + mq * kq / 128.0)
    G = np.stack([np.cos(phg), np.sin(phg), -np.sin(phg)], 2)
    out["Gtab"] = G.astype(f32)
    m1 = np.arange(32)[None, :]
    kc = np.arange(NK1)[:, None]
    ck = np.where((kc == 0) | (kc == 32), 1.0, 2.0) / 8192.0
    th2 = 2 * np.pi * m1 * kc / 64.0
    out["G1m"] = np.concatenate([ck * np.cos(th2), -ck * np.sin(th2)], 0).astype(f32)
    tc = np.zeros((32, 128), f32)
    tc[0:L // 128] = t[:, 0].reshape(L // 128, 128)
    out["tcol"] = tc
    return out


def hyena_deltas():
    hmax = math.log(1e-2) / 0.3
    hmin = math.log(1e-2) / 1.5
    return np.abs(np.linspace(hmin, hmax, HY_W, dtype=np.float32))


def build_odd_b(Lf=SEQ):
    P = Prog()
    L = SEQ
    NL1 = Lf // 128
    TW = min(512, Lf)
    d_v = P.din("v", [4, SBK, L])
    d_x1 = P.din("x1", [4, SBK, L])
    d_x2 = P.din("x2", [4, SBK, L])
    d_zf = P.din("zfT", [33, Lf])
    d_w1 = P.din("hw1", [33, 64])
    d_w2 = P.din("hw2", [64, 64])
    d_w3 = P.din("hw3", [64, 4, SBK])
    d_b1 = P.din("hb1", [64, 1])
    d_b2 = P.din("hb2", [64, 1])
    d_fr = P.din("hfr", [64, 2])
    d_bias = P.din("hbias", [2, SBK])
    d_delta = P.din("delta", [SBK])
    d_F1 = P.din("F1m", [32, 66])
    d_M = P.din("Mtab", [128, NK1, 3, 128])
    d_G = P.din("Gtab", [128, NK1, 3, 128])
    d_G1 = P.din("G1m", [66, 32])
    d_tc = P.din("tcol", [32, 128])
    o_z = P.dout("z", [4, SBK, L])
    Hd = P.dtmp("Hd", [2, 128, NK1, 2, SBK])
    Bd = P.dtmp("Bd", [128, 66, SBK], BF16)

    F1 = P.sb("F1_s", [32, 66], BF16)
    Mt = P.sb("M_s", [128, NK1, 3, 128], BF16)
    Gt = P.sb("G_s", [128, NK1, 3, 128], BF16)
    G1 = P.sb("G1_s", [66, 32], BF16)
    P.dma(F1[:], d_F1[:], [d_F1], [F1], eng="gpsimd")
    P.dma(G1[:], d_G1[:], [d_G1], [G1], eng="gpsimd")
    for i in range(3):
        ks = slice(i * 11, (i + 1) * 11)
        P.dma(Mt[:, ks], d_M[:, ks], [d_M], [Mt], eng="gpsimd")
        P.dma(Gt[:, ks], d_G[:, ks], [d_G], [Gt], eng="gpsimd")
    A = P.sb("A", [128, 66, SBK], BF16)
    Y = P.sb("Y", [128, 66, SBK], BF16)
    Bs = P.sb("Bs", [128, 66, SBK], BF16)
    Bt = [P.sb(f"Bt{i}", [66, 16, SBK], BF16) for i in range(2)]
    Hb = [P.sb(f"Hb{i}", [128, 11, 2, SBK]) for i in range(2)]
    pwt = [P.sb(f"pwt{i}", [128, 4, SBK]) for i in range(2)]
    banks = [P.ps(f"bk{i}") for i in range(8)]
    pF1, pX, pB, pY = banks[0:2], banks[2:4], banks[4:6], banks[6:8]

    S = P.nc.alloc_sbuf_tensor("scr", [128, 20480], F32)
    zb = P.view("zb", S[0:32, 0:4096].bitcast(BF16).rearrange("p (s n) -> p s n", s=SBK))
    gate = P.view("gate", S[0:32, 4096:12288].rearrange("p (s n) -> p s n", s=SBK))
    zo = P.view("zo", S[0:32, 12288:20480].rearrange("p (s n) -> p s n", s=SBK))
    zf = P.view("zf", S[0:33, 0:Lf])
    hid1 = P.view("hid1", S[0:64, 4096:8192])
    hid2 = P.view("hid2", S[0:64, 8192:10240].bitcast(BF16))
    argt = [P.view(f"argt{i}", S[0:64, 10240 + 512 * i:10752 + 512 * i]) for i in range(2)]
    tg = P.view("tg", S[0:32, 11264:15360].bitcast(BF16).rearrange("p (s n) -> p s n", s=SBK))
    Hacc = P.view("Hacc", S[:, 15360:19584].rearrange("p (k c s) -> p k c s", k=NK1, c=2))
    fviews = [zf, hid1, hid2, tg, Hacc] + argt

    w1 = P.sb("w1_s", [33, 64])
    w2 = P.sb("w2_s", [64, 64])
    w3 = P.sb("w3_s", [64, 4, SBK], BF16)
    b1 = P.sb("b1_s", [64, 1])
    b2 = P.sb("b2_s", [64, 1])
    fr = P.sb("fr_s", [64, 2])
    bias_bc = P.sb("bias_bc", [128, 2, SBK])
    delta_bc = P.sb("delta_bc", [32, SBK])
    tcol = P.sb("tcol_s", [32, 128])
    for t, d in ((w1, d_w1), (w2, d_w2), (b1, d_b1), (b2, d_b2), (fr, d_fr), (tcol, d_tc)):
        P.dma(t[:], d[:], [d], [t])
    P.dma(w3[:], d_w3[:], [d_w3], [w3], eng="gpsimd")
    P.dma(bias_bc[:], d_bias[:].rearrange("o s -> (o s)").partition_broadcast(128).rearrange("p (o s) -> p o s", o=2),
          [d_bias], [bias_bc])
    P.dma(delta_bc[:], d_delta[:].partition_broadcast(32), [d_delta], [delta_bc])
    P.dma(zf[:], d_zf[:], [d_zf], [zf])
    negpi = P.sb("negpi", [64, 1])
    P.v(lambda e: e.memset(negpi[:], -math.pi), [], [negpi])
    bf = P.sb("bf", [64, 2])
    P.v(lambda e: e.tensor_tensor(out=bf[:, 0:1], in0=b1[:], in1=fr[:, 0:1], op=ALU.mult), [b1, fr], [bf])
    P.v(lambda e: e.tensor_tensor(out=bf[:, 1:2], in0=b2[:], in1=fr[:, 1:2], op=ALU.mult), [b2, fr], [bf])
    ones32 = P.sb("ones32", [32, 128])
    P.v(lambda e: e.memset(ones32[:], 1.0), [], [ones32])
    frs = P.sb("frs", [64, 2])
    bfs = P.sb("bfs", [64, 2])
    P.v(lambda e: e.tensor_scalar(out=frs[:], in0=fr[:], scalar1=1.0 / (2.0 * math.pi), scalar2=None, op0=ALU.mult),
        [fr], [frs])
    P.v(lambda e: e.tensor_scalar(out=bfs[:], in0=bf[:], scalar1=1.0 / (2.0 * math.pi), scalar2=8.5, op0=ALU.mult,
                                  op1=ALU.add), [bf], [bfs])
    qi = P.sb("qi", [64, 512], mybir.dt.int32)
    qf = P.sb("qf", [64, 512])

    def mlp_layer(wt, K, src, layer, dst):
        for ti in range(Lf // TW):
            cs = slice(ti * TW, (ti + 1) * TW)
            p = banks[ti % 2]
            a = argt[ti % 2]
            P.mm(p[0:64, 0:TW], wt[0:K, :], src[0:K, cs], [wt, src], [p])
            P.s(lambda e, p=p, a=a: e.activation(out=a[:, 0:TW], in_=p[0:64, 0:TW], func=AF.Identity,
                                                 scale=frs[:, layer:layer + 1], bias=bfs[:, layer:layer + 1]),
                [p, frs, bfs], [a])
            P.v(lambda e, a=a: e.tensor_copy(out=qi[:, 0:TW], in_=a[:, 0:TW]), [a], [qi])
            P.v(lambda e: e.tensor_copy(out=qf[:, 0:TW], in_=qi[:, 0:TW]), [qi], [qf])
            P.v(lambda e, a=a: e.tensor_tensor(out=a[:, 0:TW], in0=a[:, 0:TW], in1=qf[:, 0:TW], op=ALU.subtract),
                [a, qf], [a])
            P.v(lambda e, a=a: e.tensor_scalar(out=qf[:, 0:TW], in0=a[:, 0:TW], scalar1=0.0, scalar2=None,
                                               op0=ALU.is_lt), [a], [qf])
            P.v(lambda e, a=a: e.tensor_tensor(out=a[:, 0:TW], in0=a[:, 0:TW], in1=qf[:, 0:TW], op=ALU.add),
                [a, qf], [a])
            P.s(lambda e, a=a, cs=cs: e.activation(out=dst[:, cs], in_=a[:, 0:TW], func=AF.Sin, bias=negpi[:, 0:1],
                                                   scale=2.0 * math.pi), [a, negpi], [dst])

    mlp_layer(w1, 33, zf, 0, hid1)
    hid1b = P.view("hid1b", S[0:64, 0:4096])
    P.fence([zf], [hid1b])
    mlp_layer(w2, 64, hid1, 1, hid1b)
    for ti in range(Lf // TW):
        cs = slice(ti * TW, (ti + 1) * TW)
        P.v(lambda e, cs=cs: e.tensor_copy(out=hid2[:, cs], in_=hid1b[:, cs]), [hid1b], [hid2])
    if NL1 < 32:
        P.v(lambda e: e.memset(tg[:], 0.0), [], [tg])

    cnt = {"f1": 0, "x": 0}

    def forward(ub, per_group):
        for s0 in range(0, SBK, 7):
            ns = min(7, SBK - s0)
            p = pF1[cnt["f1"] % 2]
            for i in range(ns):
                P.mm(p[:, i * 66:(i + 1) * 66], ub[:, s0 + i, :], F1[:], [ub, F1], [p])
            src = p[:, 0:ns * 66].rearrange("p (s k) -> p k s", s=ns)
            if cnt["f1"] % 2 == 0:
                P.s(lambda e, src=src, s0=s0, ns=ns: e.activation(out=A[:, :, s0:s0 + ns], in_=src, func=AF.Copy),
                    [p], [A])
            else:
                P.v(lambda e, src=src, s0=s0, ns=ns: e.tensor_copy(out=A[:, :, s0:s0 + ns], in_=src), [p], [A])
            cnt["f1"] += 1
        for g0 in range(0, NK1, 4):
            kk = min(4, NK1 - g0)
            px = pX[cnt["x"] % 2]
            cnt["x"] += 1
            for j in range(kk):
                k1 = g0 + j
                re = px[:, (2 * j) * SBK:(2 * j + 1) * SBK]
                im = px[:, (2 * j + 1) * SBK:(2 * j + 2) * SBK]
                P.mm(re, Mt[:, k1, 0, :], A[:, k1, :], [Mt, A], [px], start=True, stop=False)
                P.mm(re, Mt[:, k1, 2, :], A[:, NK1 + k1, :], [Mt, A], [px], start=False, stop=True)
                P.mm(im, Mt[:, k1, 0, :], A[:, NK1 + k1, :], [Mt, A], [px], start=True, stop=False)
                P.mm(im, Mt[:, k1, 1, :], A[:, k1, :], [Mt, A], [px], start=False, stop=True)
            per_group(g0, kk, px)

    def pxv(px, kk, c):
        return px[:, 0:kk * 2 * SBK].rearrange("p (k c s) -> p k c s", k=kk, c=2)[:, :, c, :]

    acc = P.sb("acc", [32, 4, SBK])
    dec = P.sb("dec", [32, 8, SBK])
    tabs = P.sb("tabs", [32, 8, SBK])
    nrm = P.sb("nrm", [128, 2, SBK])
    P.v(lambda e: e.memset(acc[:], 0.0), [], [acc])
    for o in range(2):
        for d in range(2):
            od = o * 2 + d
            for g8 in range(16):
                p = banks[(g8 % 2) + 4]
                for i in range(8):
                    n2 = g8 * 8 + i
                    P.mm(p[0:NL1, i * SBK:(i + 1) * SBK], hid2[:, n2:Lf:128], w3[:, od, :], [hid2, w3], [p])
                P.v(lambda e, g8=g8: e.tensor_tensor(
                    out=dec[0:NL1], in0=tcol[0:NL1, g8 * 8:(g8 + 1) * 8].unsqueeze(2).to_broadcast([NL1, 8, SBK]),
                    in1=delta_bc[0:NL1].unsqueeze(1).to_broadcast([NL1, 8, SBK]), op=ALU.mult), [tcol, delta_bc], [dec])
                P.s(lambda e: e.activation(out=dec[0:NL1], in_=dec[0:NL1], func=AF.Exp, scale=-1.0), [dec], [dec])
                P.v(lambda e, p=p: e.tensor_tensor(out=dec[0:NL1],
                                                   in0=p[0:NL1, 0:8 * SBK].rearrange("p (n s) -> p n s", n=8),
                                                   in1=dec[0:NL1], op=ALU.mult), [p, dec], [dec])
                P.s(lambda e, g8=g8: e.activation(out=tg[0:NL1, :, g8 * 8:(g8 + 1) * 8],
                                                  in_=dec[0:NL1].rearrange("p n s -> p s n"), func=AF.Copy), [dec], [tg])
                P.s(lambda e: e.activation(out=tabs[0:NL1], in_=dec[0:NL1], func=AF.Abs), [dec], [tabs])
                for i in range(8):
                    P.v(lambda e, i=i, od=od: e.tensor_tensor(out=acc[0:NL1, od, :], in0=acc[0:NL1, od, :],
                                                              in1=tabs[0:NL1, i, :], op=ALU.add), [acc, tabs], [acc])
            if d == 1:
                P.v(lambda e: e.memset(tg[0:1, :, 0:1], 0.0), [], [tg])
                pnm = banks[6]
                P.mm(pnm[:, 0:SBK], ones32[:], acc[:, o * 2, :], [ones32, acc], [pnm], start=True, stop=False)
                P.mm(pnm[:, 0:SBK], ones32[:], acc[:, o * 2 + 1, :], [ones32, acc], [pnm], start=False, stop=True)
                P.v(lambda e, o=o, pnm=pnm: e.reciprocal(out=nrm[:, o, :], in_=pnm[:, 0:SBK]), [pnm], [nrm])

            def grp(g0, kk, px, d=d, o=o):
                hv = Hacc[:, g0:g0 + kk, :, :]
                if d == 0:
                    P.s(lambda e: e.activation(out=hv, in_=px[:, 0:kk * 2 * SBK].rearrange("p (k c s) -> p k c s", k=kk, c=2),
                                               func=AF.Copy), [px], [Hacc])
                else:
                    P.v(lambda e: e.tensor_tensor(out=Hacc[:, g0:g0 + kk, 0, :], in0=Hacc[:, g0:g0 + kk, 0, :],
                                                  in1=pxv(px, kk, 0), op=ALU.add), [Hacc, px], [Hacc])
                    P.v(lambda e: e.tensor_tensor(out=Hacc[:, g0:g0 + kk, 1, :], in0=Hacc[:, g0:g0 + kk, 1, :],
                                                  in1=pxv(px, kk, 1), op=ALU.subtract), [Hacc, px], [Hacc])

            forward(tg, grp)
        for c in range(2):
            P.v(lambda e, c=c, o=o: e.tensor_tensor(out=Hacc[:, :, c, :], in0=Hacc[:, :, c, :],
                                                    in1=nrm[:, o, :].unsqueeze(1).to_broadcast([128, NK1, SBK]),
                                                    op=ALU.mult), [Hacc, nrm], [Hacc])
        P.v(lambda e, o=o: e.tensor_tensor(out=Hacc[:, :, 0, :], in0=Hacc[:, :, 0, :],
                                           in1=bias_bc[:, o, :].unsqueeze(1).to_broadcast([128, NK1, SBK]),
                                           op=ALU.add), [Hacc, bias_bc], [Hacc])
        P.dma(Hd[o], Hacc[:], [Hacc], [Hd])

    P.fence(fviews + [hid1b], [zb, gate, zo])
    hcnt = {"n": 0}
    for b in range(4):
        P.dma(zb[:], d_v[b].rearrange("s (a n) -> a s n", a=32), [d_v], [zb], eng="gpsimd")
        for o in range(2):
            d_g = d_x1 if o == 0 else d_x2
            P.dma(gate[:], d_g[b].rearrange("s (a n) -> a s n", a=32), [d_g], [gate])
            hbuf = {}

            def pw(g0, kk, px, o=o):
                ci = g0 // 11
                if ci not in hbuf:
                    hb = Hb[hcnt["n"] % 2]
                    hcnt["n"] += 1
                    n11 = min(11, NK1 - ci * 11)
                    P.dma(hb[:, 0:n11], Hd[o, :, ci * 11:ci * 11 + n11], [Hd], [hb])
                    hbuf[ci] = hb
                j = 0
                while j < kk:
                    k1 = g0 + j
                    ci2 = k1 // 11
                    if ci2 not in hbuf:
                        hb = Hb[hcnt["n"] % 2]
                        hcnt["n"] += 1
                        n11 = min(11, NK1 - ci2 * 11)
                        P.dma(hb[:, 0:n11], Hd[o, :, ci2 * 11:ci2 * 11 + n11], [Hd], [hb])
                        hbuf[ci2] = hb
                    hb = hbuf[ci2]
                    m = min(kk - j, (ci2 + 1) * 11 - k1)
                    l0 = k1 - ci2 * 11
                    xr = px[:, 0:kk * 2 * SBK].rearrange("p (k c s) -> p k c s", k=kk, c=2)[:, j:j + m, 0, :]
                    xi = px[:, 0:kk * 2 * SBK].rearrange("p (k c s) -> p k c s", k=kk, c=2)[:, j:j + m, 1, :]
                    hr = hb[:, l0:l0 + m, 0, :]
                    hi = hb[:, l0:l0 + m, 1, :]
                    t1, t2 = pwt[0], pwt[1]
                    P.v(lambda e, xr=xr, hr=hr, m=m: e.tensor_tensor(out=t1[:, 0:m, :], in0=xr, in1=hr, op=ALU.mult),
                        [px, hb], [t1])
                    P.v(lambda e, xi=xi, hi=hi, m=m: e.tensor_tensor(out=t2[:, 0:m, :], in0=xi, in1=hi, op=ALU.mult),
                        [px, hb], [t2])
                    P.g(lambda e, k1=k1, m=m: e.tensor_tensor(out=Y[:, k1:k1 + m, :], in0=t1[:, 0:m, :], in1=t2[:, 0:m, :],
                                                              op=ALU.subtract), [t1, t2], [Y])
                    t3, t4 = pwt[0], pwt[1]
                    P.v(lambda e, xr=xr, hi=hi, m=m: e.tensor_tensor(out=t3[:, 0:m, :], in0=xr, in1=hi, op=ALU.mult),
                        [px, hb], [t3])
                    P.v(lambda e, xi=xi, hr=hr, m=m: e.tensor_tensor(out=t4[:, 0:m, :], in0=xi, in1=hr, op=ALU.mult),
                        [px, hb], [t4])
                    P.g(lambda e, k1=k1, m=m: e.tensor_tensor(out=Y[:, NK1 + k1:NK1 + k1 + m, :], in0=t3[:, 0:m, :],
                                                              in1=t4[:, 0:m, :], op=ALU.add), [t3, t4], [Y])
                    j += m

            forward(zb, pw)
            for gi, g0 in enumerate(range(0, NK1, 4)):
                kk = min(4, NK1 - g0)
                pb = pB[gi % 2]
                for j in range(kk):
                    k1 = g0 + j
                    re = pb[:, (2 * j) * SBK:(2 * j + 1) * SBK]
                    im = pb[:, (2 * j + 1) * SBK:(2 * j + 2) * SBK]
                    P.mm(re, Gt[:, k1, 0, :], Y[:, k1, :], [Gt, Y], [pb], start=True, stop=False)
                    P.mm(re, Gt[:, k1, 2, :], Y[:, NK1 + k1, :], [Gt, Y], [pb], start=False, stop=True)
                    P.mm(im, Gt[:, k1, 0, :], Y[:, NK1 + k1, :], [Gt, Y], [pb], start=True, stop=False)
                    P.mm(im, Gt[:, k1, 1, :], Y[:, k1, :], [Gt, Y], [pb], start=False, stop=True)
                for c in range(2):
                    fn = (lambda e, c=c, g0=g0, kk=kk, pb=pb: e.activation(
                        out=Bs[:, c * NK1 + g0:c * NK1 + g0 + kk, :], in_=pxv(pb, kk, c), func=AF.Copy))
                    P.s(fn, [pb], [Bs])
            P.dma(Bd[:], Bs[:], [Bs], [Bd])
            for mc in range(8):
                bt = Bt[mc % 2]
                P.dma(bt[:], Bd[:].rearrange("m k s -> k m s")[:, mc * 16:(mc + 1) * 16, :], [Bd], [bt])
                for hh in range(2):
                    py = pY[(mc * 2 + hh) % 2]
                    m0 = mc * 16 + hh * 8
                    P.mm(py[0:32, :], G1[:], bt[:, hh * 8:(hh + 1) * 8, :], [G1, bt], [py])
                    dst = zb if o == 0 else zo
                    P.v(lambda e, py=py, m0=m0, dst=dst: e.tensor_tensor(
                        out=dst[:, :, m0:m0 + 8], in0=py[0:32, :].rearrange("p (m s) -> p s m", m=8),
                        in1=gate[:, :, m0:m0 + 8], op=ALU.mult), [py, gate], [dst])
        P.dma(o_z[b].rearrange("s (a n) -> a s n", a=32), zo[:], [zo], [o_z])
    return P.emit()


def odd_b_inputs(k, hy_full, p, o, hc):
    cs = slice(SBK * k, SBK * (k + 1))
    w3 = p["hy_w3"][o].reshape(64, 2, 2, HY_W)[:, :, :, cs].reshape(64, 4, SBK)
    m = dict(hc)
    m.update(v=np.ascontiguousarray(hy_full[:, cs, :]),
             x1=np.ascontiguousarray(hy_full[:, HY_W:2 * HY_W][:, cs, :]),
             x2=np.ascontiguousarray(hy_full[:, 2 * HY_W:3 * HY_W][:, cs, :]),
             hw1=p["hy_w1"][o], hw2=p["hy_w2"][o], hw3=np.ascontiguousarray(w3),
             hb1=np.ascontiguousarray(p["hy_b1"][o][:, None]), hb2=np.ascontiguousarray(p["hy_b2"][o][:, None]),
             hfr=np.ascontiguousarray(p["hy_freq"][o].T), hbias=np.ascontiguousarray(p["hy_bias"][o][:, cs]),
             delta=np.ascontiguousarray(hyena_deltas()[cs]))
    return m


def post_inputs(l, b, half, x, y_lat, ctx, y_ctx, modT, p, cst):
    sl = slice(half * NOWN, (half + 1) * NOWN)
    xt, yt = x[b, sl], y_lat[b, sl]
    if y_ctx is not None:
        xt = np.concatenate([xt, ctx[b]], 0)
        yt = np.concatenate([yt, y_ctx[b]], 0)
    m = dict(cst)
    m.update(xT=np.ascontiguousarray(xt.T), yT=np.ascontiguousarray(yt.T),
             modL=np.ascontiguousarray(modT[l, :, :, b]), modC=np.ascontiguousarray(modT[l, :, :, 4]),
             n1g=to_fm_cols(p["norm1_g"][l]), n2g=to_fm_cols(p["norm2_g"][l]),
             w_out=p["w_out"][l],
             w_r=np.ascontiguousarray(np.concatenate([p["moe_w_group"][l], p["moe_w_router"][l]], 1)),
             r_b=np.ascontiguousarray(np.concatenate([p["moe_b_group"][l], p["moe_b_router"][l]], 0)),
             w_gate=p["moe_w_gate"][l], w_up=p["moe_w_up"][l], w_down=p["moe_w_down"][l])
    return m


def kernel(**inputs):
    p = {k: np.ascontiguousarray(np.asarray(v, dtype=np.float32)) for k, v in inputs.items()}
    x = p["x"].copy()
    ctx = p["ctx"].copy()
    modT = compute_mods(p["c"], p["c_ctx"], p["ada_w"], p["ada_b"])
    cst = const_inputs()
    for l in range(DEPTH):
        ctx_needed = any(j % 2 == 0 for j in range(l + 1, DEPTH))
        y_lat = np.empty((BATCH, SEQ, D_MODEL), np.float32)
        y_ctx = np.empty((BATCH, CTX_LEN, D_MODEL), np.float32) if ctx_needed else None
        if l % 2 == 0:
            nc = get_prog(("em", ctx_needed), build_even_mixer, ctx_needed)
            res = run(nc, [even_mixer_inputs(l, k // 2, k % 2, x, ctx, modT, p) for k in range(NCORES)])
            for k in range(NCORES):
                b, h = k // 2, k % 2
                sl = slice(h * NOWN, (h + 1) * NOWN)
                y_lat[b, sl, :512] = res[k]["y_att"]
                y_lat[b, sl, 512:] = res[k]["y_scT"].T
                if ctx_needed and h == 0:
                    y_ctx[b, :, :512] = res[k]["y_att_c"]
                    y_ctx[b, :, 512:] = res[k]["y_scT_c"].T
        else:
            o = l // 2
            nc = get_prog(("oa", ctx_needed), build_odd_a, ctx_needed)
            res = run(nc, [odd_a_inputs(l, k // 2, k % 2, x, ctx, modT, p, ctx_needed) for k in range(NCORES)])
            hy_full = np.empty((BATCH, 1536, SEQ), np.float32)
            hy_ctx = np.zeros((BATCH, 1536, SEQ), np.float32) if ctx_needed else None
            for k in range(NCORES):
                b, h = k // 2, k % 2
                sl = slice(h * NOWN, (h + 1) * NOWN)
                hy_full[b, :, sl] = res[k]["hyT"][:, :NOWN]
                y_lat[b, sl, 512:] = res[k]["cfT"][:, :NOWN].T
                if ctx_needed and h == 0:
                    hy_ctx[b, :, :CTX_LEN] = res[k]["hyT"][:, NOWN:]
                    y_ctx[b, :, 512:] = res[k]["cfT"][:, NOWN:].T
            nc = get_prog(("ob", SEQ), build_odd_b, SEQ)
            hc = hyena_consts(SEQ)
            res = run(nc, [odd_b_inputs(k, hy_full, p, o, hc) for k in range(NCORES)])
            for k in range(NCORES):
                y_lat[:, :, SBK * k:SBK * (k + 1)] = res[k]["z"].transpose(0, 2, 1)
            if ctx_needed:
                nc = get_prog(("ob", CTX_LEN), build_odd_b, CTX_LEN)
                hc = hyena_consts(CTX_LEN)
                res = run(nc, [odd_b_inputs(k, hy_ctx, p, o, hc) for k in range(NCORES)])
                for k in range(NCORES):
                    y_ctx[:, :, SBK * k:SBK * (k + 1)] = res[k]["z"][:, :, :CTX_LEN].transpose(0, 2, 1)
        n_ctx = CTX_LEN if ctx_needed else 0
        nc = get_prog(("post", NOWN, n_ctx), build_post, NOWN, n_ctx)
        res = run(nc, [post_inputs(l, k // 2, k % 2, x, y_lat, ctx, y_ctx, modT, p, cst) for k in range(NCORES)])
        xn = np.empty_like(x)
        for k in range(NCORES):
            b, h = k // 2, k % 2
            o_t = res[k]["oT"]
            xn[b, h * NOWN:(h + 1) * NOWN] = o_t[:, :NOWN].T
            if ctx_needed and h == 0:
                ctx[b] = o_t[:, NOWN:].T
        x = xn
    return x
```
